# Optimizing a Trainium2 kernel written in Bass

```python
import jax, jax.numpy as jnp
from jax import lax
import numpy as np

D_MODEL = 1024
BATCH = 4
SEQ = 8192
DEPTH = 1

POOL_WIDTH = D_MODEL // 2
POOL_GROUPS = 4
POOL_GROUP_WIDTH = POOL_WIDTH // POOL_GROUPS
POOL_WINDOWS = (2, 4, 8, 16)
CONV_WIDTH = D_MODEL // 2
CONV_KERNEL = 31
N_BRANCHES = 2
IN_WIDTH = POOL_WIDTH + 2 * CONV_WIDTH + N_BRANCHES * D_MODEL
PEER_HEADS = 8
PEER_KEY_DIM = 256
PEER_HALF_DIM = PEER_KEY_DIM // 2
N_KEYS = 128
N_EXPERTS = N_KEYS * N_KEYS
PEER_TOPK_HALF = 16
PEER_TOPK = 16
PEER_CHUNK = 128
N_MOD = 6
EPS = 1e-6

kernel_name = "hybrid_pool_conformer_peer_adaln"


def rmsnorm(x, g):
    xf = x.astype(jnp.float32)
    y = xf * lax.rsqrt(jnp.mean(xf * xf, axis=-1, keepdims=True) + EPS)
    return (y * g.astype(jnp.float32)).astype(x.dtype)


def layernorm(x, g, b):
    xf = x.astype(jnp.float32)
    mu = jnp.mean(xf, axis=-1, keepdims=True)
    var = jnp.mean(jnp.square(xf - mu), axis=-1, keepdims=True)
    y = (xf - mu) * lax.rsqrt(var + EPS)
    return (y * g.astype(jnp.float32) + b.astype(jnp.float32)).astype(x.dtype)


def causal_window_mean(xg, w):
    s = xg.shape[1]
    xf = xg.astype(jnp.float32)
    cs = jnp.cumsum(xf, axis=1)
    shifted = jnp.pad(cs, ((0, 0), (w, 0), (0, 0)))[:, :s]
    count = jnp.minimum(jnp.arange(1, s + 1), w).astype(jnp.float32)[None, :, None]
    return ((cs - shifted) / count).astype(xg.dtype)


def pool_branch(p, pool_w, pool_scale, pool_up):
    b, s, _ = p.shape
    pooled = jnp.concatenate(
        [causal_window_mean(p[..., i * POOL_GROUP_WIDTH:(i + 1) * POOL_GROUP_WIDTH], w)
         for i, w in enumerate(POOL_WINDOWS)], axis=-1)
    mixed = (pooled - p).reshape(b, s, POOL_GROUPS, POOL_GROUP_WIDTH)
    mixed = jnp.einsum('bsgc,gcd->bsgd', mixed, pool_w).reshape(b, s, POOL_WIDTH)
    return (mixed * pool_scale) @ pool_up


def conformer_branch(a, dw_kernel, dw_bias, ln_g, ln_b, conv_out):
    glu = a[..., :CONV_WIDTH] * jax.nn.sigmoid(a[..., CONV_WIDTH:])
    y = lax.conv_general_dilated(
        glu, dw_kernel[:, None, :].astype(glu.dtype), window_strides=(1,),
        padding=[(CONV_KERNEL - 1, 0)], dimension_numbers=('NWC', 'WIO', 'NWC'),
        feature_group_count=CONV_WIDTH) + dw_bias
    y = layernorm(y, ln_g, ln_b)
    y = y * jax.nn.sigmoid(y)
    return y @ conv_out


def peer(h, w_query, keys_1, keys_2, expert_u, expert_v):
    b, s, d = h.shape
    flat = h.reshape(b * s // PEER_CHUNK, PEER_CHUNK, d)

    def chunk_fn(hc):
        q = (hc @ w_query).reshape(PEER_CHUNK, PEER_HEADS, PEER_KEY_DIM)
        s1 = jnp.einsum('chk,nk->chn', q[..., :PEER_HALF_DIM], keys_1)
        s2 = jnp.einsum('chk,nk->chn', q[..., PEER_HALF_DIM:], keys_2)
        v1, i1 = lax.top_k(s1, PEER_TOPK_HALF)
        v2, i2 = lax.top_k(s2, PEER_TOPK_HALF)
        cand = (v1[..., :, None] + v2[..., None, :]).reshape(PEER_CHUNK, PEER_HEADS, -1)
        cand_idx = (i1[..., :, None] * N_KEYS + i2[..., None, :]).reshape(PEER_CHUNK, PEER_HEADS, -1)
        top_s, pos = lax.top_k(cand, PEER_TOPK)
        experts = jnp.take_along_axis(cand_idx, pos, axis=-1)
        gate = jax.nn.softmax(top_s.astype(jnp.float32), axis=-1).astype(hc.dtype)
        u = jnp.take(expert_u, experts, axis=0)
        act = jax.nn.gelu(jnp.einsum('chkd,cd->chk', u, hc))
        v = jnp.take(expert_v, experts, axis=0)
        return jnp.einsum('chk,chkd->cd', gate * act, v)

    return lax.map(chunk_fn, flat).reshape(b, s, d)


def setup_inputs(seed: int = 0) -> dict:
    key = jax.random.key(seed)
    ks = jax.random.split(key, 24)

    def nrm(k, shape, scale):
        return jax.random.normal(k, shape, jnp.float32) * scale

    def gain(k, shape):
        return 1.0 + 0.05 * jax.random.normal(k, shape, jnp.float32)

    L = DEPTH
    return {
        "x": nrm(ks[0], (BATCH, SEQ, D_MODEL), 1.0),
        "c": nrm(ks[1], (BATCH, D_MODEL), 1.0),
        "ada_w": nrm(ks[2], (L, D_MODEL, N_MOD * D_MODEL), 0.5 * D_MODEL ** -0.5),
        "ada_b": nrm(ks[3], (L, N_MOD * D_MODEL), 0.01),
        "norm1_g": gain(ks[4], (L, D_MODEL)),
        "w_in": nrm(ks[5], (L, D_MODEL, IN_WIDTH), D_MODEL ** -0.5),
        "b_in": nrm(ks[6], (L, IN_WIDTH), 0.01),
        "pool_w": nrm(ks[7], (L, POOL_GROUPS, POOL_GROUP_WIDTH, POOL_GROUP_WIDTH), POOL_GROUP_WIDTH ** -0.5),
        "pool_scale": gain(ks[8], (L, POOL_WIDTH)),
        "pool_up": nrm(ks[9], (L, POOL_WIDTH, D_MODEL), POOL_WIDTH ** -0.5),
        "dw_kernel": nrm(ks[10], (L, CONV_KERNEL, CONV_WIDTH), CONV_KERNEL ** -0.5),
        "dw_bias": nrm(ks[11], (L, CONV_WIDTH), 0.01),
        "conv_ln_g": gain(ks[12], (L, CONV_WIDTH)),
        "conv_ln_b": nrm(ks[13], (L, CONV_WIDTH), 0.01),
        "conv_out": nrm(ks[14], (L, CONV_WIDTH, D_MODEL), CONV_WIDTH ** -0.5),
        "w_out": nrm(ks[15], (L, D_MODEL, D_MODEL), D_MODEL ** -0.5),
        "norm2_g": gain(ks[16], (L, D_MODEL)),
        "w_query": nrm(ks[17], (L, D_MODEL, PEER_HEADS * PEER_KEY_DIM), D_MODEL ** -0.5),
        "sub_keys_1": nrm(ks[18], (L, N_KEYS, PEER_HALF_DIM), PEER_HALF_DIM ** -0.5),
        "sub_keys_2": nrm(ks[19], (L, N_KEYS, PEER_HALF_DIM), PEER_HALF_DIM ** -0.5),
        "expert_u": nrm(ks[20], (L, N_EXPERTS, D_MODEL), D_MODEL ** -0.5),
        "expert_v": nrm(ks[21], (L, N_EXPERTS, D_MODEL), PEER_HEADS ** -0.5),
        "final_g": gain(ks[22], (D_MODEL,)),
    }


def reference(x, c, ada_w, ada_b, norm1_g, w_in, b_in, pool_w, pool_scale, pool_up,
              dw_kernel, dw_bias, conv_ln_g, conv_ln_b, conv_out, w_out, norm2_g,
              w_query, sub_keys_1, sub_keys_2, expert_u, expert_v, final_g):
    c_act = jax.nn.silu(c)
    for l in range(DEPTH):
        mod = c_act @ ada_w[l] + ada_b[l]
        shift1, scale1, gate1, shift2, scale2, gate2 = [
            m[:, None, :] for m in jnp.split(mod, N_MOD, axis=-1)]

        h = rmsnorm(x, norm1_g[l]) * (1.0 + scale1) + shift1
        proj = h @ w_in[l] + b_in[l]
        p = proj[..., :POOL_WIDTH]
        a = proj[..., POOL_WIDTH:POOL_WIDTH + 2 * CONV_WIDTH]
        g = proj[..., POOL_WIDTH + 2 * CONV_WIDTH:]
        y_pool = pool_branch(p, pool_w[l], pool_scale[l], pool_up[l])
        y_conv = conformer_branch(a, dw_kernel[l], dw_bias[l], conv_ln_g[l], conv_ln_b[l], conv_out[l])
        merged = jax.nn.sigmoid(g[..., :D_MODEL]) * y_pool + jax.nn.sigmoid(g[..., D_MODEL:]) * y_conv
        x = x + gate1 * (merged @ w_out[l])

        h2 = rmsnorm(x, norm2_g[l]) * (1.0 + scale2) + shift2
        x = x + gate2 * peer(h2, w_query[l], sub_keys_1[l], sub_keys_2[l], expert_u[l], expert_v[l])
    return rmsnorm(x, final_g)
```

```python
import numpy as np
from contextlib import ExitStack
import concourse.bass as bass
import concourse.mybir as mybir
from concourse.bass_utils import run_bass_kernel_spmd

F32 = mybir.dt.float32
BF16 = mybir.dt.bfloat16
U32 = mybir.dt.uint32
AF = mybir.ActivationFunctionType
ALU = mybir.AluOpType
AX = mybir.AxisListType

TB = 256
STEP_COUNT = [0, 0]
PCAP = 5
NG = 80
NSLOT = 5
A_TOTAL = 300
A_STEPS = 2
EPS = 1e-6

_cols = {}
_off = 0
for _n, _w in [("n1g", 8), ("n2g", 8), ("bin", 28), ("psc", 4), ("dw", 124), ("dwb", 4), ("lng", 4),
               ("lnb", 4), ("adab", 48), ("c", 8), ("flag", 1), ("invc", 64), ("keys", 256),
               ("ident", 128), ("iota", 128), ("iota16", 16)]:
    _cols[_n] = (_off, _w)
    _off += _w
NCOL = _off


class _Rec:
    def __init__(self):
        self.call = None

    def __getattr__(self, name):
        def f(*args, **kw):
            self.call = (name, args, kw)
            return None
        return f


_WRITE_KEYS = ("out", "accum_out", "ap")


def _is_ap(v):
    return hasattr(v, "ap") and hasattr(v, "offset") and hasattr(v, "space")


def _ap_range(ap):
    pat = list(ap.ap)
    esz = mybir.dt.size(ap.dtype)
    space = str(ap.space)
    if "PSUM" in space:
        return (ap.name, 0, 1 << 30)
    if "DRAM" in space:
        return None
    pstride = pat[0][0]
    colo = ap.offset % pstride if pstride > 0 else ap.offset
    hi = colo + sum(abs(st) * (cn - 1) for st, cn in pat[1:]) + 1
    return (ap.name, colo * esz, hi * esz)


class Sched:
    def __init__(self, nc, stack):
        self.nc = nc
        self.engs = {'pe': nc.tensor, 'act': nc.scalar, 'dve': nc.vector, 'pool': nc.gpsimd, 'sp': nc.sync}
        self.sems = {}
        self.cnt = {}
        self.waited = {k: {} for k in self.engs}
        self.stack = stack
        self.last = {}
        self.hist = {}
        self.pending = {k: [] for k in self.engs}
        for k in self.engs:
            self.sems[k] = stack.enter_context(nc.semaphore("s_" + k))
            self.cnt[k] = 0
            self.last[k] = None

    def new_sem(self, name):
        self.sems[name] = self.stack.enter_context(self.nc.semaphore("d_" + name))
        self.cnt[name] = 0
        return name

    def wait(self, eng, ev):
        if ev is None:
            return
        key, val = ev
        if self.waited[eng].get(key, 0) >= val:
            return
        self.engs[eng].wait_ge(self.sems[key], val)
        self.waited[eng][key] = val

    def _hazard_deps(self, eng, reads, writes):
        deps = []
        for (rng, is_w) in [(r, False) for r in reads] + [(w, True) for w in writes]:
            if rng is None:
                continue
            name, lo, hi = rng
            for ent in self.hist.get(name, ()):
                if ent[1] <= lo or ent[0] >= hi:
                    continue
                if not (ent[3] or is_w):
                    continue
                ev = ent[2]
                if ent[4] == 'pe' and eng == 'pe':
                    continue
                if isinstance(ev, list):
                    raise AssertionError("hazard against unsignaled op on %s: %s vs %s" % (ent[4], name, eng))
                deps.append(ev)
        return deps

    def _record(self, eng, reads, writes, ev):
        for (rng, is_w) in [(r, False) for r in reads] + [(w, True) for w in writes]:
            if rng is None:
                continue
            name, lo, hi = rng
            lst = self.hist.setdefault(name, [])
            if is_w:
                lst[:] = [e for e in lst if not (e[0] >= lo and e[1] <= hi and not isinstance(e[2], list))]
            else:
                if eng != "dma":
                    lst[:] = [e for e in lst if not ((not e[3]) and e[4] == eng and e[0] >= lo and e[1] <= hi
                                                     and not isinstance(e[2], list))]
            ent = [lo, hi, ev, is_w, eng]
            lst.append(ent)
            if isinstance(ev, list):
                ev.append(ent)

    def op(self, eng, fn, deps=(), signal=True):
        rec = _Rec()
        fn(rec)
        name, args, kw = rec.call
        reads, writes = [], []
        if name == "memset":
            writes.append(_ap_range(args[0] if args else kw["ap"]))
        else:
            assert not args or name in ("matmul",), (name, args)
            if name == "matmul" and args:
                kw = dict(kw)
                kw["out"] = args[0]
                args = ()
            for k, v in kw.items():
                if _is_ap(v):
                    (writes if k in _WRITE_KEYS else reads).append(_ap_range(v))
        hz = self._hazard_deps(eng, reads, writes)
        for d in list(deps) + hz:
            self.wait(eng, d)
        ins = getattr(self.engs[eng], name)(*args, **kw)
        if signal:
            self.cnt[eng] += 1
            ins.then_inc(self.sems[eng], 1)
            ev = (eng, self.cnt[eng])
            self.last[eng] = ev
            for ent in self.pending[eng]:
                ent[2] = ev
            self.pending[eng] = []
            self._record(eng, reads, writes, ev)
            return ev
        self._record(eng, reads, writes, self.pending[eng])
        return None

    def dma(self, eng, out, in_, semname, deps=()):
        reads = [_ap_range(in_)]
        writes = [_ap_range(out)]
        hz = self._hazard_deps(eng, reads, writes)
        for d in list(deps) + hz:
            self.wait(eng, d)
        ins = self.engs[eng].dma_start(out=out, in_=in_)
        self.cnt[semname] += 16
        ins.then_inc(self.sems[semname], 16)
        ev = (semname, self.cnt[semname])
        self._record("dma", reads, writes, ev)
        return ev

    def barrier(self, engs=('pe', 'act', 'dve')):
        evs = [self.last[e] for e in engs]
        for e in engs:
            for ev in evs:
                if ev is not None and ev[0] != e:
                    self.wait(e, ev)


def build_nc(NTOK, stream_in=None, rec_out=None):
    NB = NTOK // TB
    RECORD = stream_in is None
    nc = bass.Bass("TRN2", target_bir_lowering=False)
    x_d = nc.dram_tensor("x", [NTOK, 1024], F32, kind="ExternalInput").ap()
    xh_d = nc.dram_tensor("xh", [32, 1024], F32, kind="ExternalInput").ap()
    sp_d = nc.dram_tensor("smallp", [128, NCOL], F32, kind="ExternalInput").ap()
    rows_d = nc.dram_tensor("rows", [1, 3072], F32, kind="ExternalInput").ap()
    adaw_d = nc.dram_tensor("adaw", [12, 128, 4096], F32, kind="ExternalInput").ap()
    wsrc_d = nc.dram_tensor("wsrc", [NG, 128, 4096], F32, kind="ExternalInput").ap()
    wscr_d = nc.dram_tensor("wscr", [NG, 128, 4096], BF16, kind="Internal").ap()
    out_d = nc.dram_tensor("out", [NTOK, 1024], F32, kind="ExternalOutput").ap()

    with ExitStack() as st:
        S = Sched(nc, st)
        sb = lambda name, shape, dt: st.enter_context(nc.sbuf_tensor(name, shape, dt))
        ps_t = lambda name, shape, dt: st.enter_context(nc.psum_tensor(name, shape, dt))

        RA_EL = 24896
        smallp = sb("smallp_sb", [128, NCOL], F32)
        rows_bc = sb("rows_bc", [128, 3072], F32)
        ring = sb("ring", [128, NSLOT, 4096], BF16)
        R = sb("Rbig", [128, 32768], BF16)
        RA = sb("RA", [128, RA_EL], BF16)
        dring = sb("dring", [128, 8, 128], BF16)
        xbuf = sb("xbuf", [128, 2, 2, 1024], F32)
        xn = sb("xn", [128, 2, 1024], BF16)
        hTb = sb("hTb", [128, 2, 8, TB], BF16)
        modT = sb("modT", [128, 48], F32)
        gs = sb("gs", [128, 4, 8], F32)
        stat = sb("stat", [128, 16], F32)
        epst = sb("epst", [128, 1], F32)
        cact_f = sb("cact_f", [128, 8], F32)
        cact_bf = sb("cact_bf", [128, 8, 2], BF16)
        ones512 = sb("ones512", [128, 128], F32)
        identb = sb("identb", [128, 128], BF16)
        iota_b = sb("iota_b", [128, 128], BF16)
        aT = sb("aT", [128, TB], F32)
        bT = sb("bT", [128, TB], F32)
        gT = sb("gT", [128, TB], F32)
        Lb = sb("Lb", [128, 4, 128], BF16)
        Rb = sb("Rb", [128, 4, 128], BF16)
        ga = sb("ga", [128, 2, TB], BF16)
        Wt = sb("Wt", [128, 3, TB], BF16)
        pmarg = sb("pmarg", [128, 4, 16], F32)
        gmarg = sb("gmarg", [128, 4, 32], BF16)

        def col(name, i=0, w=1):
            o, _ = _cols[name]
            return smallp[:, o + i:o + i + w]

        ident_f = smallp[:, _cols["ident"][0]:_cols["ident"][0] + 128]
        iota_f = smallp[:, _cols["iota"][0]:_cols["iota"][0] + 128]
        iota16 = smallp[:, _cols["iota16"][0]:_cols["iota16"][0] + 16]
        keysT = smallp[:, _cols["keys"][0]:_cols["keys"][0] + 256].rearrange("p (h n) -> p h n", h=2)
        invc = smallp[:, _cols["invc"][0]:_cols["invc"][0] + 64].rearrange("p (g t) -> p g t", g=4)
        flag = col("flag")
        fg_bc = rows_bc[:, 0:1024]
        gate1_bc = rows_bc[:, 1024:2048]
        gate2_bc = rows_bc[:, 2048:3072]
        g1s = gs[:, 0, :]
        g2s = gs[:, 2, :]
        gtmp = gs[:, 3, :]
        sh1 = modT[:, 0:8]
        sh2 = modT[:, 24:32]

        class Carver:
            def __init__(self):
                self.off = 0

            def take(self, nel_bf16, dt, shape_str=None, **kw):
                a = RA[:, self.off:self.off + nel_bf16]
                self.off += nel_bf16
                assert self.off <= RA_EL, self.off
                if dt == F32:
                    a = a.bitcast(F32)
                elif dt == U32:
                    a = a.bitcast(U32)
                if shape_str:
                    a = a.rearrange(shape_str, **kw)
                return a

        ca = Carver()
        sgT = ca.take(4096, BF16, "p (n t) -> p n t", n=16)
        pT = ca.take(2176, F32, "p (g t) -> p g t", g=4)
        gluT = ca.take(1152, BF16, "p (g t) -> p g t", g=4)
        sigA = ca.take(1024, BF16, "p (g t) -> p g t", g=4)
        mixedT = ca.take(1024, BF16, "p (g t) -> p g t", g=4)
        mix2T = ca.take(1024, BF16, "p (g t) -> p g t", g=4)
        swT = ca.take(1024, BF16, "p (g t) -> p g t", g=4)
        mergedT = ca.take(2048, BF16, "p (g t) -> p g t", g=8)
        yT = ca.take(2048, F32, "p (g t) -> p g t", g=4)
        ysq = ca.take(2048, F32, "p (g t) -> p g t", g=4)
        m1_off = ca.off
        m1 = ca.take(2048, BF16, "p (g t) -> p g t", g=8)
        pp0 = ca.take(544, F32)
        pp1 = ca.take(544, F32)
        lnm = ca.take(512, F32)
        lnv = ca.take(512, F32)
        lnr = ca.take(512, F32)
        lnt = ca.take(512, F32)
        tmp512 = ca.take(1024, F32)
        junk = ca.take(1024, BF16)
        a1_end = ca.off
        ch = Carver()
        ch.off = m1_off
        xnh = ch.take(1024, BF16)
        hTh = ch.take(256, BF16, "p (k t) -> p k t", k=8)
        sigAh = ch.take(256, F32, "p (g t) -> p g t", g=4)
        gluh_t = ch.take(64, F32)
        assert ch.off <= m1_off + 2048
        cact_rep = RA[:, 0:1024].rearrange("p (k m) -> p k m", k=8)
        cb = Carver()
        qT = cb.take(2048, F32, "p (r q t) -> p r q t", r=2, q=2)
        S_sb = cb.take(4096, F32, "p (g n) -> p g n", g=16)
        work = cb.take(4096, F32, "p (g n) -> p g n", g=16)
        v16 = cb.take(512, F32, "p (g k) -> p g k", g=16)
        idxu = cb.take(512, U32, "p (g k) -> p g k", g=16)
        idxf = cb.take(512, F32, "p (g k) -> p g k", g=16)
        tops = cb.take(256, F32, "p (h k) -> p h k", h=8)
        posu = cb.take(256, U32, "p (h k) -> p h k", h=8)
        apu = cb.take(256, U32, "p (h k) -> p h k", h=8)
        bpu = cb.take(256, U32, "p (h k) -> p h k", h=8)
        apf = cb.take(256, F32, "p (h k) -> p h k", h=8)
        bpf = cb.take(256, F32, "p (h k) -> p h k", h=8)
        areal = cb.take(256, F32, "p (h k) -> p h k", h=8)
        breal = cb.take(256, F32, "p (h k) -> p h k", h=8)
        dd = cb.take(256, F32, "p (h k) -> p h k", h=8)
        ee = cb.take(256, F32, "p (h k) -> p h k", h=8)
        gate = cb.take(256, F32, "p (h k) -> p h k", h=8)
        zz = cb.take(32, F32)
        assert cb.off <= a1_end - 2048, (cb.off, a1_end)
        S_flat = S_sb.rearrange("p g n -> p (g n)")
        W_flat = work.rearrange("p g n -> p (g n)")
        cand = S_flat.rearrange("p (h a b) -> p h a b", h=8, a=16)
        work2 = W_flat.rearrange("p (h n) -> p h n", h=8)
        eq4 = S_flat.rearrange("p (h k a) -> p h k a", h=8, k=16)
        prod4 = W_flat.rearrange("p (h k a) -> p h k a", h=8, k=16)
        xh_sb = xbuf[:, 1, 0, :]
        GT = R[:, :].rearrange("p (t c) -> p t c", c=128)

        bankA = [ps_t("bankA%d" % i, [128, 512], F32) for i in range(4)]
        accB = [ps_t("accB%d" % i, [128, 512], F32) for i in range(4)]
        slotsE = [0, 1]
        slotsA = [2, 3]
        rot = {"E": 0, "A": 0}

        def ps_alloc(kind):
            lst = slotsE if kind == "E" else slotsA
            i = lst[rot[kind] % len(lst)]
            rot[kind] += 1
            return bankA[i]

        for i in range(NSLOT):
            S.new_sem("ld%d" % i)
            S.new_sem("lds%d" % i)
            S.new_sem("wb%d" % i)
        for n in ["setup", "setup2", "xld0", "xld1", "xst", "xh"] + ["cv%d" % i for i in range(16)]:
            S.new_sem(n)

        stream = [] if RECORD else list(stream_in)
        st_state = {"issued": 0, "cur": 0}
        free_slots = list(range(NSLOT))
        slot_of = {}
        load_ev = {}
        wb_ev = {}

        cv_ev = {}

        def issue_one(si):
            item = stream[si]
            slot = free_slots.pop(0)
            slot_of[si] = slot
            dst = ring[:, slot, :]
            if item[0] == 'ada':
                ev = S.dma('pool', dst, adaw_d[item[1]], "ld%d" % slot)
            elif item[1] == 0 and item[2] < 16:
                ev = S.dma('pool', dst, wsrc_d[item[2]], "ld%d" % slot)
                wb_ev[item[2]] = S.dma('sp', wscr_d[item[2]], dst, "wb%d" % slot, deps=[ev])
            elif item[1] == 0:
                ev = S.dma('sp', dst, wscr_d[item[2]], "lds%d" % slot, deps=[cv_ev[(item[2] - 16) // 4]])
            else:
                ev = S.dma('sp', dst, wscr_d[item[2]], "lds%d" % slot,
                           deps=[wb_ev[item[2]]] if (item[1] == 1 and item[2] < 16) else [])
            load_ev[si] = ev

        def try_issue():
            while st_state["issued"] < len(stream) and free_slots:
                issue_one(st_state["issued"])
                st_state["issued"] += 1

        def acquire(item):
            si = st_state["cur"]
            st_state["cur"] += 1
            if RECORD:
                stream.append(item)
                if not free_slots:
                    raise AssertionError("ring exhausted in record mode")
                issue_one(si)
                st_state["issued"] = si + 1
            else:
                assert stream[si] == item, (si, stream[si], item)
                if si not in load_ev:
                    try_issue()
                assert si in load_ev, ("ring slot not available", si, item)
            return ring[:, slot_of[si], :], load_ev[si], si

        def release(si):
            free_slots.append(slot_of[si])
            if not RECORD:
                try_issue()

        def mm(out, lhsT, rhs, start, stop, deps=(), signal=False):
            return S.op('pe', lambda e: e.matmul(out=out, lhsT=lhsT, rhs=rhs, start=start, stop=stop), deps, signal)

        A = lambda **kw: (lambda e: e.activation(**kw))

        S.dma('act', smallp[:], sp_d, "setup2")
        e_setup = S.dma('act', rows_bc[:], rows_d.partition_broadcast(128), "setup")
        if not RECORD:
            try_issue()
        for i in range(16):
            cv_ev[i] = S.dma('pool', wscr_d[16 + 4 * i:20 + 4 * i], wsrc_d[16 + 4 * i:20 + 4 * i], "cv%d" % i)
        xld_ev = {0: S.dma('act', xbuf[:, 0, :, :], x_d[0:TB, :].rearrange("(t p) d -> p t d", p=128), "xld0")}
        e_xh = S.dma('act', xh_sb[0:32, :], xh_d, "xh")
        xst_ev = {}

        S.op('dve', lambda e: e.tensor_copy(out=identb[:], in_=ident_f), deps=[e_setup])
        S.op('dve', lambda e: e.tensor_copy(out=iota_b[:], in_=iota_f), deps=[e_setup])
        S.op('dve', lambda e: e.memset(ap=ones512[:], constant=1.0 / 512))
        S.op('dve', lambda e: e.memset(ap=epst[:], constant=EPS))
        S.op('act', A(out=cact_f[:], in_=col("c", 0, 8), func=AF.Silu), deps=[e_setup])
        S.op('dve', lambda e: e.tensor_copy(out=cact_bf[:], in_=cact_f[:].unsqueeze(2).to_broadcast([128, 8, 2])))
        S.op('dve', lambda e: e.tensor_copy(out=cact_rep, in_=cact_f[:].unsqueeze(2).to_broadcast([128, 8, 128])))

        modps = ps_alloc("A")
        for j in range(12):
            slot, lev, si = acquire(('ada', j))
            sl = slot.rearrange("p (k n) -> p k n", k=8)
            last = None
            for dt_ in range(4):
                for kt in range(8):
                    last = mm(modps[:, 2 * (j * 4 + dt_):2 * (j * 4 + dt_) + 2], sl[:, kt, dt_ * 128:(dt_ + 1) * 128],
                              cact_bf[:, kt, :], kt == 0, kt == 7, deps=[lev], signal=(kt == 7 and dt_ == 3))
            if j in (4, 5, 10, 11):
                half = j % 2
                which = 1 if j < 8 else 2
                bank = accB[(0 if j < 8 else 2) + half]
                for kt in range(8):
                    last = mm(bank[:, :], cact_rep[:, kt, :], sl[:, kt, :], kt == 0, kt == 7, signal=(kt == 7))
                dst = rows_bc[:, which * 1024 + half * 512: which * 1024 + (half + 1) * 512]
                S.op('dve', lambda e, dst=dst, bank=bank: e.tensor_tensor(out=dst, in0=dst, in1=bank[:, :], op=ALU.add))
            release(si)
        S.op('dve', lambda e: e.tensor_tensor(out=modT[:], in0=modps[:, 0:96].rearrange("p (j two) -> p j two", two=2)[:, :, 0],
                                              in1=col("adab", 0, 48), op=ALU.add))
        S.op('dve', lambda e: e.tensor_single_scalar(out=gtmp, in_=modT[:, 8:16], scalar=1.0, op=ALU.add))
        S.op('dve', lambda e: e.tensor_tensor(out=g1s, in0=gtmp, in1=col("n1g", 0, 8), op=ALU.mult))
        S.op('dve', lambda e: e.tensor_single_scalar(out=gtmp, in_=modT[:, 32:40], scalar=1.0, op=ALU.add))
        S.op('dve', lambda e: e.tensor_tensor(out=g2s, in0=gtmp, in1=col("n2g", 0, 8), op=ALU.mult))

        def rms_and_transpose(xt_aps, npart, dst_hT_fn, gsc, shf, xn_aps, scol=0):
            for ti, xt in enumerate(xt_aps):
                ssc = stat[0:npart, scol + ti:scol + ti + 1]
                sdc = stat[0:npart, 4 + scol + ti:5 + scol + ti]
                rsc = stat[0:npart, 8 + scol + ti:9 + scol + ti]
                S.op('act', A(out=junk[0:npart, :], in_=xt, func=AF.Square, accum_out=ssc))
                S.op('act', A(out=sdc, in_=ssc, func=AF.Sqrt, scale=1.0 / 1024, bias=epst[0:npart, :]))
                yield
                S.op('dve', lambda e: e.reciprocal(out=rsc, in_=sdc))
                xna = xn_aps[ti]
                S.op('dve', lambda e: e.tensor_scalar(out=xna, in0=xt, scalar1=rsc, scalar2=None, op0=ALU.mult))
                yield
                for hf in range(2):
                    pst = ps_alloc("A")[:, 0:256].bitcast(BF16)
                    for q in range(4):
                        kt = hf * 4 + q
                        S.op('pe', lambda e, kt=kt, q=q: e.transpose(
                            out=pst[:, q * 128:q * 128 + npart], in_=xna[:, kt * 128:(kt + 1) * 128],
                            identity=identb[0:npart, 0:npart]), signal=(q == 3))
                    yield
                    for q in range(4):
                        kt = hf * 4 + q
                        S.op('dve', lambda e, kt=kt, q=q: e.tensor_scalar(
                            out=dst_hT_fn(kt, ti), in0=pst[:, q * 128:q * 128 + npart], scalar1=gsc[:, kt:kt + 1],
                            scalar2=shf[:, kt:kt + 1], op0=ALU.mult, op1=ALU.add))
                    yield

        dr = {"i": 0}

        def stageA(b):
            par = b % 2
            xt = [xbuf[:, par, 0, :], xbuf[:, par, 1, :]]
            hT = hTb[:, par, :, :]
            if b == 0:
                yield from rms_and_transpose([xh_sb[0:32, :]], 32, lambda kt, ti: hTh[:, kt, :], g1s, sh1, [xnh[0:32, :]], scol=2)
            yield from rms_and_transpose(xt, 128, lambda kt, ti: hT[:, kt, ti * 128:(ti + 1) * 128], g1s, sh1,
                                         [xn[:, 0, :], xn[:, 1, :]])
            if b > 0:
                S.op('dve', lambda e: e.tensor_copy(out=pT[:, :, 0:16], in_=pmarg[:]))
                S.op('dve', lambda e: e.tensor_copy(out=gluT[:, :, 0:32], in_=gmarg[:]))
            for piece in range(7):
                slot, lev, si = acquire(('w', b, piece))
                sl = slot.rearrange("p (k n) -> p k n", k=8)
                for nl in range(4):
                    if piece == 0:
                        tn = nl
                    elif piece == 1:
                        tn = 8 + nl
                    elif piece == 2:
                        tn = 4 + nl
                    else:
                        tn = 12 + (piece - 3) * 4 + nl
                    bcol = col("bin", tn)
                    if b == 0 and piece < 3:
                        psh = ps_alloc("A")
                        for kt in range(8):
                            mm(psh[:, 0:32], sl[:, kt, nl * 128:(nl + 1) * 128], hTh[:, kt, :], kt == 0, kt == 7,
                               deps=[lev], signal=(kt == 7))
                        if piece == 0:
                            S.op('dve', lambda e: e.tensor_scalar(out=pT[:, nl, 0:16], in0=psh[:, 16:32], scalar1=bcol,
                                                                  scalar2=flag, op0=ALU.add, op1=ALU.mult))
                        elif piece == 1:
                            S.op('act', A(out=sigAh[:, nl, :], in_=psh[:, 0:32], func=AF.Sigmoid, bias=bcol))
                        else:
                            S.op('dve', lambda e: e.scalar_tensor_tensor(out=gluh_t, in0=psh[:, 0:32], scalar=bcol,
                                                                          in1=sigAh[:, nl, :], op0=ALU.add, op1=ALU.mult))
                            S.op('dve', lambda e: e.tensor_scalar(out=gluT[:, nl, 0:32], in0=gluh_t, scalar1=flag,
                                                                  scalar2=None, op0=ALU.mult))
                    psm = ps_alloc("A")[:, 0:256]
                    for kt in range(8):
                        mm(psm, sl[:, kt, nl * 128:(nl + 1) * 128], hT[:, kt, :], kt == 0, kt == 7, deps=[lev], signal=(kt == 7))
                    if piece == 0:
                        S.op('act', A(out=pT[:, nl, 16:272], in_=psm, func=AF.Identity, bias=bcol))
                    elif piece == 1:
                        S.op('act', A(out=sigA[:, nl, :], in_=psm, func=AF.Sigmoid, bias=bcol))
                    elif piece == 2:
                        S.op('dve', lambda e: e.scalar_tensor_tensor(out=gluT[:, nl, 32:288], in0=psm, scalar=bcol,
                                                                      in1=sigA[:, nl, :], op0=ALU.add, op1=ALU.mult))
                    else:
                        n = (piece - 3) * 4 + nl
                        S.op('act', A(out=sgT[:, n, :], in_=psm, func=AF.Sigmoid, bias=bcol))
                    yield
                release(si)
            if b + 1 < NB:
                S.op('dve', lambda e: e.tensor_copy(out=pmarg[:], in_=pT[:, :, 256:272]))
                S.op('dve', lambda e: e.tensor_copy(out=gmarg[:], in_=gluT[:, :, 256:288]))
            for g in range(4):
                src = pT[:, g, :]
                lo = 0
                bufs = [pp0, pp1]
                bi = 0
                for stp in [1, 2, 4, 8][:g + 1]:
                    nlo = lo + stp
                    dstb = bufs[bi]
                    S.op('dve', lambda e, src=src, dstb=dstb, nlo=nlo, stp=stp: e.tensor_tensor(
                        out=dstb[:, nlo:272], in0=src[:, nlo:272], in1=src[:, nlo - stp:272 - stp], op=ALU.add))
                    src = dstb
                    lo = nlo
                    bi ^= 1
                    if stp >= 2:
                        yield
                w = 2 ** (g + 1)
                S.op('dve', lambda e, src=src, g=g, w=w: e.scalar_tensor_tensor(
                    out=mixedT[:, g, :], in0=src[:, 16:272], scalar=1.0 / w, in1=pT[:, g, 16:272], op0=ALU.mult, op1=ALU.subtract))
                if b == 0:
                    S.op('dve', lambda e, src=src, g=g: e.tensor_tensor(out=lnt[:, 0:16], in0=src[:, 16:32], in1=invc[:, g, :], op=ALU.mult))
                    S.op('dve', lambda e, g=g: e.tensor_tensor(out=mixedT[:, g, 0:16], in0=lnt[:, 0:16], in1=pT[:, g, 16:32], op=ALU.subtract))
                yield
            slot, lev, si = acquire(('w', b, 7))
            for g in range(4):
                psm = ps_alloc("A")[:, 0:256]
                mm(psm, slot[:, g * 128:(g + 1) * 128], mixedT[:, g, :], True, True, deps=[lev], signal=True)
                S.op('act', A(out=mix2T[:, g, :], in_=psm, func=AF.Identity, scale=col("psc", g)))
                yield
            release(si)
            taps = [(c, k) for c in range(4) for k in range(31)]
            groups = [taps[i:i + 4] for i in range(0, 124, 4)]
            tile_of = {}

            def build_diag(g):
                for (c, k) in groups[g]:
                    di = dr["i"] % 8
                    dr["i"] += 1
                    tile_of[(c, k)] = di
                    S.op('dve', lambda e, c=c, k=k, di=di: e.tensor_scalar(
                        out=dring[:, di, :], in0=identb[:], scalar1=col("dw", c * 31 + k), scalar2=None, op0=ALU.mult))

            build_diag(0)
            yield
            cps = {}
            for g in range(len(groups)):
                if g + 1 < len(groups):
                    build_diag(g + 1)
                for (c, k) in groups[g]:
                    if k == 0:
                        cps[c] = ps_alloc("A")[:, 0:256]
                    mm(cps[c], dring[:, tile_of[(c, k)], :], gluT[:, c, 2 + k:2 + k + 256], k == 0, k == 30, signal=True)
                    if k == 30:
                        S.op('act', A(out=yT[:, c, :], in_=cps[c], func=AF.Identity, bias=col("dwb", c)))
                        S.op('act', A(out=ysq[:, c, :], in_=cps[c], func=AF.Square, bias=col("dwb", c)))
                yield
            psmean = ps_alloc("A")[:, 0:256]
            for c in range(4):
                mm(psmean, ones512[:], yT[:, c, :], c == 0, c == 3, signal=(c == 3))
            psq = ps_alloc("A")[:, 0:256]
            for c in range(4):
                mm(psq, ones512[:], ysq[:, c, :], c == 0, c == 3, signal=(c == 3))
            yield
            S.op('act', A(out=lnm, in_=psmean, func=AF.Copy))
            yield
            S.op('dve', lambda e: e.tensor_tensor(out=lnt, in0=lnm, in1=lnm, op=ALU.mult))
            S.op('dve', lambda e: e.tensor_tensor(out=lnv, in0=psq, in1=lnt, op=ALU.subtract))
            yield
            S.op('act', A(out=lnv, in_=lnv, func=AF.Sqrt, bias=epst[:, :]))
            yield
            S.op('dve', lambda e: e.reciprocal(out=lnr, in_=lnv))
            yield
            lnts = [lnt, pp0[:, 0:256]]
            for c in range(4):
                lt_ = lnts[c % 2]
                S.op('dve', lambda e, c=c: e.tensor_tensor(out=lt_, in0=yT[:, c, :], in1=lnm, op=ALU.subtract))
                S.op('dve', lambda e: e.tensor_tensor(out=lt_, in0=lt_, in1=lnr, op=ALU.mult))
                yield
                S.op('act', A(out=swT[:, c, :], in_=lt_, func=AF.Silu, scale=col("lng", c), bias=col("lnb", c)))
                yield
            slot, lev, si = acquire(('w', b, 8))
            sl = slot.rearrange("p (k n) -> p k n", k=4)
            for n in range(8):
                psm = ps_alloc("A")[:, 0:256]
                for kt in range(4):
                    mm(psm, sl[:, kt, n * 128:(n + 1) * 128], mix2T[:, kt, :], kt == 0, kt == 3, deps=[lev], signal=(kt == 3))
                S.op('dve', lambda e, n=n: e.tensor_tensor(out=m1[:, n, :], in0=psm, in1=sgT[:, n, :], op=ALU.mult))
                yield
            release(si)
            slot, lev, si = acquire(('w', b, 9))
            sl = slot.rearrange("p (k n) -> p k n", k=4)
            for n in range(8):
                psm = ps_alloc("A")[:, 0:256]
                for kt in range(4):
                    mm(psm, sl[:, kt, n * 128:(n + 1) * 128], swT[:, kt, :], kt == 0, kt == 3, deps=[lev], signal=(kt == 3))
                S.op('dve', lambda e, n=n: e.tensor_tensor(out=lnt, in0=psm, in1=sgT[:, 8 + n, :], op=ALU.mult))
                S.op('dve', lambda e, n=n: e.tensor_tensor(out=mergedT[:, n, :], in0=lnt, in1=m1[:, n, :], op=ALU.add))
                yield
            release(si)
            for half in range(2):
                slot, lev, si = acquire(('w', b, 10 + half))
                sl = slot.rearrange("p (k n) -> p k n", k=8)
                for tt in range(2):
                    pso = ps_alloc("A")
                    for kt in range(8):
                        mm(pso[:, :], mergedT[:, kt, tt * 128:(tt + 1) * 128], sl[:, kt, :], kt == 0, kt == 7, deps=[lev], signal=(kt == 7))
                    S.op('dve', lambda e, half=half: e.tensor_tensor(
                        out=tmp512, in0=pso[:, :], in1=gate1_bc[:, half * 512:(half + 1) * 512], op=ALU.mult))
                    xs = xt[tt][:, half * 512:(half + 1) * 512]
                    S.op('dve', lambda e, xs=xs: e.tensor_tensor(out=xs, in0=xs, in1=tmp512, op=ALU.add))
                    yield
                release(si)
            yield from rms_and_transpose(xt, 128, lambda kt, ti: hT[:, kt, ti * 128:(ti + 1) * 128], g2s, sh2,
                                         [xn[:, 0, :], xn[:, 1, :]])
            def emit_scores(h, r):
                for tt in range(2):
                    pss = ps_alloc("A")[:, 0:256]
                    for half in range(2):
                        mm(pss[:, half * 128:(half + 1) * 128], qT[:, r, half, tt * 128:(tt + 1) * 128], keysT[:, half, :],
                           True, True, signal=(half == 1))
                    dstS = (S_sb if tt == 0 else work)[:, 2 * h:2 * h + 2, :]
                    S.op('act', A(out=dstS, in_=pss.rearrange("p (g n) -> p g n", g=2), func=AF.Copy))

            prev_q = None
            for piece in range(4):
                slot, lev, si = acquire(('w', b, 12 + piece))
                sl = slot.rearrange("p (k n) -> p k n", k=8)
                for hh in range(2):
                    h = piece * 2 + hh
                    r = h % 2
                    pq = ps_alloc("A")
                    for half in range(2):
                        nl = hh * 2 + half
                        for kt in range(8):
                            mm(pq[:, half * 256:(half + 1) * 256], sl[:, kt, nl * 128:(nl + 1) * 128], hT[:, kt, :],
                               kt == 0, kt == 7, deps=[lev], signal=(kt == 7 and half == 1))
                    S.op('act', A(out=qT[:, r, :, :], in_=pq[:, :].rearrange("p (q t) -> p q t", q=2), func=AF.Copy))
                    if prev_q is not None:
                        yield
                        emit_scores(*prev_q)
                    prev_q = (h, r)
                    yield
                release(si)
            emit_scores(*prev_q)
            yield
            pend_tr = []
            for tt in range(2):
                if tt == 1:
                    for q4 in range(4):
                        S.op('act', A(out=S_flat[:, q4 * 512:(q4 + 1) * 512], in_=W_flat[:, q4 * 512:(q4 + 1) * 512], func=AF.Copy))
                        yield 'D'
                for gi in range(16):
                    S.op('dve', lambda e, gi=gi: e.max(out=v16[:, gi, 0:8], in_=S_sb[:, gi, :]))
                    if gi % 4 == 3:
                        yield 'D'
                for gi in range(16):
                    S.op('dve', lambda e, gi=gi: e.max_index(out=idxu[:, gi, 0:8], in_max=v16[:, gi, 0:8], in_values=S_sb[:, gi, :]))
                    if gi % 4 == 3:
                        yield 'D'
                if tt == 1 and pend_tr:
                    pend_tr.pop(0)()
                    yield 'D'
                for gi in range(16):
                    S.op('dve', lambda e, gi=gi: e.match_replace(out=S_sb[:, gi, :], in_to_replace=v16[:, gi, 0:8],
                                                                 in_values=S_sb[:, gi, :], imm_value=-1e30))
                    if gi % 4 == 3:
                        yield 'D'
                for gi in range(16):
                    S.op('dve', lambda e, gi=gi: e.max(out=v16[:, gi, 8:16], in_=S_sb[:, gi, :]))
                    if gi % 4 == 3:
                        yield 'D'
                for gi in range(16):
                    S.op('dve', lambda e, gi=gi: e.max_index(out=idxu[:, gi, 8:16], in_max=v16[:, gi, 8:16], in_values=S_sb[:, gi, :]))
                    if gi % 4 == 3:
                        yield 'D'
                S.op('dve', lambda e: e.tensor_copy(out=idxf, in_=idxu))
                v4 = v16.rearrange("p (h two) k -> p h two k", two=2)
                i4 = idxf.rearrange("p (h two) k -> p h two k", two=2)
                yield 'D'
                for hp in range(4):
                    hs_ = slice(2 * hp, 2 * hp + 2)
                    S.op('dve', lambda e: e.tensor_tensor(
                        out=cand[:, hs_], in0=v4[:, hs_, 0, :].unsqueeze(3).to_broadcast([128, 2, 16, 16]),
                        in1=v4[:, hs_, 1, :].unsqueeze(2).to_broadcast([128, 2, 16, 16]), op=ALU.add))
                    yield 'D'
                candf = cand.rearrange("p h a b -> p h (a b)")
                for h in range(8):
                    S.op('dve', lambda e, h=h: e.max(out=tops[:, h, 0:8], in_=candf[:, h, :]))
                    if h % 4 == 3:
                        yield 'D'
                for h in range(8):
                    S.op('dve', lambda e, h=h: e.max_index(out=posu[:, h, 0:8], in_max=tops[:, h, 0:8], in_values=candf[:, h, :]))
                    if h % 4 == 3:
                        yield 'D'
                for h in range(8):
                    S.op('dve', lambda e, h=h: e.match_replace(out=candf[:, h, :], in_to_replace=tops[:, h, 0:8],
                                                               in_values=candf[:, h, :], imm_value=-1e30))
                    if h % 4 == 3:
                        yield 'D'
                for h in range(8):
                    S.op('dve', lambda e, h=h: e.max(out=tops[:, h, 8:16], in_=candf[:, h, :]))
                    if h % 4 == 3:
                        yield 'D'
                for h in range(8):
                    S.op('dve', lambda e, h=h: e.max_index(out=posu[:, h, 8:16], in_max=tops[:, h, 8:16], in_values=candf[:, h, :]))
                    if h % 4 == 3:
                        yield 'D'
                S.op('dve', lambda e: e.tensor_single_scalar(out=apu, in_=posu, scalar=4, op=ALU.logical_shift_right))
                S.op('dve', lambda e: e.tensor_single_scalar(out=bpu, in_=posu, scalar=15, op=ALU.bitwise_and))
                S.op('dve', lambda e: e.tensor_copy(out=apf, in_=apu))
                S.op('dve', lambda e: e.tensor_copy(out=bpf, in_=bpu))
                yield 'D'
                io4 = iota16.unsqueeze(1).unsqueeze(1).to_broadcast([128, 8, 16, 16])
                eqv, prv = eq4, eq4
                for (posf, side, dstr) in [(apf, 0, areal), (bpf, 1, breal)]:
                    for hp in range(4):
                        hs_ = slice(2 * hp, 2 * hp + 2)
                        io4h = iota16.unsqueeze(1).unsqueeze(1).to_broadcast([128, 2, 16, 16])
                        S.op('dve', lambda e: e.tensor_tensor(out=eqv[:, hs_], in0=posf[:, hs_].unsqueeze(3).to_broadcast([128, 2, 16, 16]),
                                                              in1=io4h, op=ALU.is_equal))
                        yield 'D'
                        S.op('dve', lambda e: e.tensor_tensor(out=eqv[:, hs_], in0=eqv[:, hs_],
                                                              in1=i4[:, hs_, side, :].unsqueeze(2).to_broadcast([128, 2, 16, 16]), op=ALU.mult))
                        yield 'D'
                        S.op('dve', lambda e: e.tensor_reduce(out=dstr[:, hs_], in_=eqv[:, hs_], axis=AX.X, op=ALU.add))
                        yield 'D'
                S.op('dve', lambda e: e.tensor_tensor(out=dd, in0=tops, in1=tops[:, :, 0:1].to_broadcast([128, 8, 16]), op=ALU.subtract))
                yield 'D'
                S.op('act', A(out=ee, in_=dd, func=AF.Exp))
                yield 'D'
                S.op('dve', lambda e: e.tensor_reduce(out=zz[:, 0:8], in_=ee, axis=AX.X, op=ALU.add))
                S.op('dve', lambda e: e.reciprocal(out=zz[:, 8:16], in_=zz[:, 0:8]))
                S.op('dve', lambda e: e.tensor_tensor(out=gate, in0=ee, in1=zz[:, 8:16].unsqueeze(2).to_broadcast([128, 8, 16]), op=ALU.mult))
                yield 'D'
                def emit_tr(tt=tt):
                    for (srcv, dstv) in [(areal, aT), (breal, bT), (gate, gT)]:
                        pst_ = ps_alloc("A")[:, 0:128]
                        S.op('pe', lambda e, srcv=srcv: e.transpose(out=pst_, in_=srcv.rearrange("p h k -> p (h k)"), identity=ident_f))
                        S.op('act', A(out=dstv[:, tt * 128:(tt + 1) * 128], in_=pst_, func=AF.Copy))
                pend_tr.append(emit_tr)
                yield 'D'
            for f_ in pend_tr:
                f_()
            yield 'D'

        def gbuild(b):
            psm = None
            for t in range(TB):
                ls = t % 4
                S.op('dve', lambda e: e.tensor_scalar(out=Rb[:, ls, :], in0=iota_b[:], scalar1=bT[:, t:t + 1], scalar2=None, op0=ALU.is_equal))
                S.op('dve', lambda e: e.tensor_scalar(out=Lb[:, ls, :], in0=iota_b[:], scalar1=aT[:, t:t + 1], scalar2=gT[:, t:t + 1],
                                                      op0=ALU.is_equal, op1=ALU.mult))
                if t % 4 == 0:
                    psm = bankA[t // 4 % 4]
                mm(psm[:, (t % 4) * 128:(t % 4 + 1) * 128], Rb[:, ls, :], Lb[:, ls, :], True, True, signal=True)
                if t % 4 == 3:
                    t0 = t - 3
                    S.op('act', A(out=GT[:, t0:t0 + 4, :], in_=psm[:, :].rearrange("p (t c) -> p t c", t=4), func=AF.Copy))

        def expert(b, genA):
            hT = hTb[:, b % 2, :, :]
            ev_held = {}

            def emit_U(c):
                gq, ci = c // 4, c % 4
                if ci == 0:
                    ev_held[('U', gq)] = acquire(('w', b, 16 + 2 * gq))
                slot, lev, si = ev_held[('U', gq)]
                su = slot.rearrange("p (c k e) -> p c k e", c=4, k=8)
                psm = ps_alloc("E")[:, 0:256]
                for kt in range(8):
                    mm(psm, su[:, ci, kt, :], hT[:, kt, :], kt == 0, kt == 7, deps=[lev], signal=(kt == 7))
                if ci == 3:
                    release(si)
                g_i = c % 2
                S.op('act', A(out=ga[:, g_i, :], in_=psm, func=AF.Gelu_apprx_tanh))
                w_i = c % 3
                S.op('dve', lambda e: e.tensor_tensor(out=Wt[:, w_i, :], in0=ga[:, g_i, :], in1=GT[:, :, c], op=ALU.mult))

            def emit_V(c):
                gq, ci = c // 4, c % 4
                if ci == 0:
                    ev_held[('V', gq)] = acquire(('w', b, 17 + 2 * gq))
                slot, lev, si = ev_held[('V', gq)]
                sv = slot.rearrange("p (c d) -> p c d", c=4)
                w_i = c % 3
                for tt in range(2):
                    for half in range(2):
                        mm(accB[tt * 2 + half][:, :], Wt[:, w_i, tt * 128:(tt + 1) * 128], sv[:, ci, half * 512:(half + 1) * 512],
                           c == 0, c == 127, deps=[lev], signal=(tt == 1 and half == 1))
                if ci == 3:
                    release(si)

            SKEW = 1
            alive = genA is not None
            for c in range(128 + SKEW):
                if c < 128:
                    emit_U(c)
                if c - SKEW >= 0:
                    emit_V(c - SKEW)
                if alive and c >= 2:
                    pp_, pd_ = 0, 0
                    dcap = 2 if (c % 2 == 0) else 1
                    while pp_ < PCAP and pd_ < dcap:
                        try:
                            tag = next(genA)
                        except StopIteration:
                            alive = False
                            break
                        if tag == 'D':
                            pd_ += 1
                            STEP_COUNT[1] += 1
                        else:
                            pp_ += 1
                            STEP_COUNT[0] += 1
            if alive:
                for _ in genA:
                    pass

        def final(b):
            par = b % 2
            xt = [xbuf[:, par, 0, :], xbuf[:, par, 1, :]]
            for tt in range(2):
                for half in range(2):
                    S.op('dve', lambda e: e.tensor_tensor(out=tmp512, in0=accB[tt * 2 + half][:, :],
                                                          in1=gate2_bc[:, half * 512:(half + 1) * 512], op=ALU.mult))
                    xs = xt[tt][:, half * 512:(half + 1) * 512]
                    S.op('dve', lambda e: e.tensor_tensor(out=xs, in0=xs, in1=tmp512, op=ALU.add))
            for tt in range(2):
                ssc = stat[:, tt:tt + 1]
                sdc = stat[:, 4 + tt:5 + tt]
                rsc = stat[:, 8 + tt:9 + tt]
                S.op('act', A(out=junk[:, :], in_=xt[tt], func=AF.Square, accum_out=ssc))
                S.op('act', A(out=sdc, in_=ssc, func=AF.Sqrt, scale=1.0 / 1024, bias=epst[:, :]))
                S.op('dve', lambda e: e.reciprocal(out=rsc, in_=sdc))
                S.op('dve', lambda e: e.scalar_tensor_tensor(out=xt[tt], in0=xt[tt], scalar=rsc, in1=fg_bc, op0=ALU.mult, op1=ALU.mult))
            xst_ev[b] = S.dma('act', out_d[b * TB:(b + 1) * TB, :].rearrange("(t p) d -> p t d", p=128), xbuf[:, par, :, :], "xst")

        gen0 = stageA(0)
        for _ in gen0:
            pass
        if NB > 1:
            S.dma('act', xbuf[:, 1, :, :], x_d[TB:2 * TB, :].rearrange("(t p) d -> p t d", p=128), "xld1")
        for b in range(NB):
            gbuild(b)
            expert(b, stageA(b + 1) if b + 1 < NB else None)
            final(b)
            if b + 2 < NB:
                S.dma('act', xbuf[:, b % 2, :, :], x_d[(b + 2) * TB:(b + 3) * TB, :].rearrange("(t p) d -> p t d", p=128),
                      "xld%d" % (b % 2))
        S.wait('act', xst_ev[NB - 1])
        for g in sorted(wb_ev)[-NSLOT:]:
            S.wait('sp', wb_ev[g])
    if rec_out is not None:
        rec_out.extend(stream)
    return nc


def _prep_shared(inp):
    f = np.float32
    w_in = np.asarray(inp["w_in"], f)[0]
    groups = np.zeros((NG, 128, 4096), f)

    def kt_layout(w, ktn):
        n = w.shape[1]
        return np.ascontiguousarray(w.reshape(ktn, 128, n).transpose(1, 0, 2)).reshape(128, ktn * n)

    pieces = [w_in[:, 0:512], w_in[:, 1024:1536], w_in[:, 512:1024]] + [w_in[:, 1536 + i * 512:1536 + (i + 1) * 512] for i in range(4)]
    for i, p in enumerate(pieces):
        groups[i] = kt_layout(p, 8)
    pw = np.asarray(inp["pool_w"], f)[0]
    groups[7, :, 0:512] = pw.transpose(1, 0, 2).reshape(128, 512)
    groups[8] = kt_layout(np.asarray(inp["pool_up"], f)[0], 4)
    groups[9] = kt_layout(np.asarray(inp["conv_out"], f)[0], 4)
    wo = np.asarray(inp["w_out"], f)[0]
    groups[10] = kt_layout(wo[:, 0:512], 8)
    groups[11] = kt_layout(wo[:, 512:1024], 8)
    wq = np.asarray(inp["w_query"], f)[0]
    for i in range(4):
        groups[12 + i] = kt_layout(wq[:, i * 512:(i + 1) * 512], 8)
    U = np.asarray(inp["expert_u"], f)[0]
    V = np.asarray(inp["expert_v"], f)[0]
    U5 = U.reshape(32, 4, 128, 8, 128)
    groups[16::2] = U5.transpose(0, 4, 1, 3, 2).reshape(32, 128, 4096)
    V4 = V.reshape(32, 4, 128, 1024)
    groups[17::2] = V4.transpose(0, 2, 1, 3).reshape(32, 128, 4096)
    ada_w = np.asarray(inp["ada_w"], f)[0]
    adaw = np.zeros((12, 128, 4096), f)
    for j in range(12):
        adaw[j] = kt_layout(ada_w[:, j * 512:(j + 1) * 512], 8)
    ada_b = np.asarray(inp["ada_b"], f)[0]
    rows = np.concatenate([np.asarray(inp["final_g"], f), ada_b[2048:3072], ada_b[5120:6144]])[None, :]

    sp = np.zeros((128, NCOL), f)

    def put(name, arr):
        o, w = _cols[name]
        sp[:, o:o + w] = arr.reshape(128, w)

    tcol = lambda vec, n: np.asarray(vec, f).reshape(n, 128).T
    put("n1g", tcol(inp["norm1_g"][0], 8))
    put("n2g", tcol(inp["norm2_g"][0], 8))
    put("bin", tcol(inp["b_in"][0], 28))
    put("psc", tcol(inp["pool_scale"][0], 4))
    dw = np.asarray(inp["dw_kernel"], f)[0]
    put("dw", dw.reshape(31, 4, 128).transpose(2, 1, 0).reshape(128, 124))
    put("dwb", tcol(inp["dw_bias"][0], 4))
    put("lng", tcol(inp["conv_ln_g"][0], 4))
    put("lnb", tcol(inp["conv_ln_b"][0], 4))
    put("adab", tcol(ada_b, 48))
    k1 = np.asarray(inp["sub_keys_1"], f)[0]
    k2 = np.asarray(inp["sub_keys_2"], f)[0]
    put("keys", np.concatenate([k1.T, k2.T], axis=1))
    put("ident", np.eye(128, dtype=f))
    put("iota", np.tile(np.arange(128, dtype=f), (128, 1)))
    put("iota16", np.tile(np.arange(16, dtype=f), (128, 1)))
    return groups, adaw, rows, sp


def _core_inputs(inp, shared, batch, start, ntok):
    f = np.float32
    groups, adaw, rows, sp0 = shared
    x = np.asarray(inp["x"], f)
    sp = sp0.copy()
    o, w = _cols["c"]
    sp[:, o:o + w] = np.asarray(inp["c"], f)[batch].reshape(8, 128).T
    o, _ = _cols["flag"]
    first = (start == 0)
    sp[:, o] = 0.0 if first else 1.0
    o, w = _cols["invc"]
    iv = np.zeros((4, 16), f)
    for g in range(4):
        wd = 2 ** (g + 1)
        for t in range(16):
            iv[g, t] = 1.0 / (min(t + 1, wd) if first else wd)
    sp[:, o:o + w] = np.tile(iv.reshape(1, 64), (128, 1))
    if first:
        xh = np.zeros((32, 1024), f)
    else:
        xh = np.ascontiguousarray(x[batch, start - 32:start])
    return {"x": np.ascontiguousarray(x[batch, start:start + ntok]), "xh": xh, "smallp": sp, "rows": rows,
            "adaw": adaw, "wsrc": groups}


_NC_CACHE = {}


def run(inp, ntok, cores):
    if ntok not in _NC_CACHE:
        rec = []
        build_nc(ntok, None, rec)
        _NC_CACHE[ntok] = build_nc(ntok, rec)
    nc = _NC_CACHE[ntok]
    shared = _prep_shared(inp)
    in_maps = [_core_inputs(inp, shared, bt, stt, ntok) for (bt, stt) in cores]
    res = run_bass_kernel_spmd(nc, in_maps, core_ids=list(range(len(cores))))
    return [r["out"] for r in res.results]


def kernel(**inputs):
    x = np.asarray(inputs["x"])
    B, SEQ, D = x.shape
    ntok = SEQ // 2
    cores = [(bt, h * ntok) for bt in range(B) for h in range(2)]
    outs = run(inputs, ntok, cores)
    out = np.zeros((B, SEQ, D), np.float32)
    for (bt, stt), o in zip(cores, outs):
        out[bt, stt:stt + ntok] = o
    return out
```

```python
import numpy as np
from contextlib import ExitStack
import concourse.bass as bass
import concourse.mybir as mybir
from concourse.bass_utils import run_bass_kernel_spmd

F32 = mybir.dt.float32
BF16 = mybir.dt.bfloat16
U32 = mybir.dt.uint32
AF = mybir.ActivationFunctionType
ALU = mybir.AluOpType
AX = mybir.AxisListType

TB = 256
STEP_COUNT = [0, 0]
PCAP = 6
NG = 80
NSLOT = 5
A_TOTAL = 300
A_STEPS = 2
EPS = 1e-6

_cols = {}
_off = 0
for _n, _w in [("n1g", 8), ("n2g", 8), ("bin", 28), ("psc", 4), ("dw", 124), ("dwb", 4), ("lng", 4),
               ("lnb", 4), ("adab", 48), ("c", 8), ("flag", 1), ("invc", 64), ("keys", 256),
               ("ident", 128), ("iota", 128), ("iota16", 16)]:
    _cols[_n] = (_off, _w)
    _off += _w
NCOL = _off


class _Rec:
    def __init__(self):
        self.call = None

    def __getattr__(self, name):
        def f(*args, **kw):
            self.call = (name, args, kw)
            return None
        return f


_WRITE_KEYS = ("out", "accum_out", "ap")


def _is_ap(v):
    return hasattr(v, "ap") and hasattr(v, "offset") and hasattr(v, "space")


def _ap_range(ap):
    pat = list(ap.ap)
    esz = mybir.dt.size(ap.dtype)
    space = str(ap.space)
    if "PSUM" in space:
        return (ap.name, 0, 1 << 30)
    if "DRAM" in space:
        return None
    pstride = pat[0][0]
    colo = ap.offset % pstride if pstride > 0 else ap.offset
    hi = colo + sum(abs(st) * (cn - 1) for st, cn in pat[1:]) + 1
    return (ap.name, colo * esz, hi * esz)


class Sched:
    def __init__(self, nc, stack):
        self.nc = nc
        self.engs = {'pe': nc.tensor, 'act': nc.scalar, 'dve': nc.vector, 'pool': nc.gpsimd, 'sp': nc.sync}
        self.sems = {}
        self.cnt = {}
        self.waited = {k: {} for k in self.engs}
        self.stack = stack
        self.last = {}
        self.hist = {}
        self.pending = {k: [] for k in self.engs}
        for k in self.engs:
            self.sems[k] = stack.enter_context(nc.semaphore("s_" + k))
            self.cnt[k] = 0
            self.last[k] = None

    def new_sem(self, name):
        self.sems[name] = self.stack.enter_context(self.nc.semaphore("d_" + name))
        self.cnt[name] = 0
        return name

    def wait(self, eng, ev):
        if ev is None:
            return
        key, val = ev
        if self.waited[eng].get(key, 0) >= val:
            return
        self.engs[eng].wait_ge(self.sems[key], val)
        self.waited[eng][key] = val

    def _hazard_deps(self, eng, reads, writes):
        deps = []
        for (rng, is_w) in [(r, False) for r in reads] + [(w, True) for w in writes]:
            if rng is None:
                continue
            name, lo, hi = rng
            for ent in self.hist.get(name, ()):
                if ent[1] <= lo or ent[0] >= hi:
                    continue
                if not (ent[3] or is_w):
                    continue
                ev = ent[2]
                if ent[4] == 'pe' and eng == 'pe':
                    continue
                if isinstance(ev, list):
                    raise AssertionError("hazard against unsignaled op on %s: %s vs %s" % (ent[4], name, eng))
                deps.append(ev)
        return deps

    def _record(self, eng, reads, writes, ev):
        for (rng, is_w) in [(r, False) for r in reads] + [(w, True) for w in writes]:
            if rng is None:
                continue
            name, lo, hi = rng
            lst = self.hist.setdefault(name, [])
            if is_w:
                lst[:] = [e for e in lst if not (e[0] >= lo and e[1] <= hi and not isinstance(e[2], list))]
            else:
                if eng != "dma":
                    lst[:] = [e for e in lst if not ((not e[3]) and e[4] == eng and e[0] >= lo and e[1] <= hi
                                                     and not isinstance(e[2], list))]
            ent = [lo, hi, ev, is_w, eng]
            lst.append(ent)
            if isinstance(ev, list):
                ev.append(ent)

    def op(self, eng, fn, deps=(), signal=True):
        rec = _Rec()
        fn(rec)
        name, args, kw = rec.call
        reads, writes = [], []
        if name == "memset":
            writes.append(_ap_range(args[0] if args else kw["ap"]))
        else:
            assert not args or name in ("matmul",), (name, args)
            if name == "matmul" and args:
                kw = dict(kw)
                kw["out"] = args[0]
                args = ()
            for k, v in kw.items():
                if _is_ap(v):
                    (writes if k in _WRITE_KEYS else reads).append(_ap_range(v))
        hz = self._hazard_deps(eng, reads, writes)
        for d in list(deps) + hz:
            self.wait(eng, d)
        ins = getattr(self.engs[eng], name)(*args, **kw)
        if signal:
            self.cnt[eng] += 1
            ins.then_inc(self.sems[eng], 1)
            ev = (eng, self.cnt[eng])
            self.last[eng] = ev
            for ent in self.pending[eng]:
                ent[2] = ev
            self.pending[eng] = []
            self._record(eng, reads, writes, ev)
            return ev
        self._record(eng, reads, writes, self.pending[eng])
        return None

    def dma(self, eng, out, in_, semname, deps=()):
        reads = [_ap_range(in_)]
        writes = [_ap_range(out)]
        hz = self._hazard_deps(eng, reads, writes)
        for d in list(deps) + hz:
            self.wait(eng, d)
        ins = self.engs[eng].dma_start(out=out, in_=in_)
        self.cnt[semname] += 16
        ins.then_inc(self.sems[semname], 16)
        ev = (semname, self.cnt[semname])
        self._record("dma", reads, writes, ev)
        return ev

    def barrier(self, engs=('pe', 'act', 'dve')):
        evs = [self.last[e] for e in engs]
        for e in engs:
            for ev in evs:
                if ev is not None and ev[0] != e:
                    self.wait(e, ev)


def build_nc(NTOK, stream_in=None, rec_out=None):
    NB = NTOK // TB
    RECORD = stream_in is None
    nc = bass.Bass("TRN2", target_bir_lowering=False)
    x_d = nc.dram_tensor("x", [NTOK, 1024], F32, kind="ExternalInput").ap()
    xh_d = nc.dram_tensor("xh", [32, 1024], F32, kind="ExternalInput").ap()
    sp_d = nc.dram_tensor("smallp", [128, NCOL], F32, kind="ExternalInput").ap()
    rows_d = nc.dram_tensor("rows", [1, 3072], F32, kind="ExternalInput").ap()
    adaw_d = nc.dram_tensor("adaw", [12, 128, 4096], F32, kind="ExternalInput").ap()
    wsrc_d = nc.dram_tensor("wsrc", [NG, 128, 4096], F32, kind="ExternalInput").ap()
    wscr_d = nc.dram_tensor("wscr", [NG, 128, 4096], BF16, kind="Internal").ap()
    out_d = nc.dram_tensor("out", [NTOK, 1024], F32, kind="ExternalOutput").ap()

    with ExitStack() as st:
        S = Sched(nc, st)
        sb = lambda name, shape, dt: st.enter_context(nc.sbuf_tensor(name, shape, dt))
        ps_t = lambda name, shape, dt: st.enter_context(nc.psum_tensor(name, shape, dt))

        RA_EL = 24896
        smallp = sb("smallp_sb", [128, NCOL], F32)
        rows_bc = sb("rows_bc", [128, 3072], F32)
        ring = sb("ring", [128, NSLOT, 4096], BF16)
        R = sb("Rbig", [128, 32768], BF16)
        RA = sb("RA", [128, RA_EL], BF16)
        dring = sb("dring", [128, 8, 128], BF16)
        xbuf = sb("xbuf", [128, 2, 2, 1024], F32)
        xn = sb("xn", [128, 2, 1024], BF16)
        hTb = sb("hTb", [128, 2, 8, TB], BF16)
        modT = sb("modT", [128, 48], F32)
        gs = sb("gs", [128, 4, 8], F32)
        stat = sb("stat", [128, 16], F32)
        epst = sb("epst", [128, 1], F32)
        cact_f = sb("cact_f", [128, 8], F32)
        cact_bf = sb("cact_bf", [128, 8, 2], BF16)
        ones512 = sb("ones512", [128, 128], F32)
        identb = sb("identb", [128, 128], BF16)
        iota_b = sb("iota_b", [128, 128], BF16)
        aT = sb("aT", [128, TB], F32)
        bT = sb("bT", [128, TB], F32)
        gT = sb("gT", [128, TB], F32)
        Lb = sb("Lb", [128, 4, 128], BF16)
        Rb = sb("Rb", [128, 4, 128], BF16)
        ga = sb("ga", [128, 2, TB], BF16)
        Wt = sb("Wt", [128, 3, TB], BF16)
        pmarg = sb("pmarg", [128, 4, 16], F32)
        gmarg = sb("gmarg", [128, 4, 32], BF16)

        def col(name, i=0, w=1):
            o, _ = _cols[name]
            return smallp[:, o + i:o + i + w]

        ident_f = smallp[:, _cols["ident"][0]:_cols["ident"][0] + 128]
        iota_f = smallp[:, _cols["iota"][0]:_cols["iota"][0] + 128]
        iota16 = smallp[:, _cols["iota16"][0]:_cols["iota16"][0] + 16]
        keysT = smallp[:, _cols["keys"][0]:_cols["keys"][0] + 256].rearrange("p (h n) -> p h n", h=2)
        invc = smallp[:, _cols["invc"][0]:_cols["invc"][0] + 64].rearrange("p (g t) -> p g t", g=4)
        flag = col("flag")
        fg_bc = rows_bc[:, 0:1024]
        gate1_bc = rows_bc[:, 1024:2048]
        gate2_bc = rows_bc[:, 2048:3072]
        g1s = gs[:, 0, :]
        g2s = gs[:, 2, :]
        gtmp = gs[:, 3, :]
        sh1 = modT[:, 0:8]
        sh2 = modT[:, 24:32]

        class Carver:
            def __init__(self):
                self.off = 0

            def take(self, nel_bf16, dt, shape_str=None, **kw):
                a = RA[:, self.off:self.off + nel_bf16]
                self.off += nel_bf16
                assert self.off <= RA_EL, self.off
                if dt == F32:
                    a = a.bitcast(F32)
                elif dt == U32:
                    a = a.bitcast(U32)
                if shape_str:
                    a = a.rearrange(shape_str, **kw)
                return a

        ca = Carver()
        sgT = ca.take(4096, BF16, "p (n t) -> p n t", n=16)
        pT = ca.take(2176, F32, "p (g t) -> p g t", g=4)
        gluT = ca.take(1152, BF16, "p (g t) -> p g t", g=4)
        sigA = ca.take(1024, BF16, "p (g t) -> p g t", g=4)
        mixedT = ca.take(1024, BF16, "p (g t) -> p g t", g=4)
        mix2T = ca.take(1024, BF16, "p (g t) -> p g t", g=4)
        swT = ca.take(1024, BF16, "p (g t) -> p g t", g=4)
        mergedT = ca.take(2048, BF16, "p (g t) -> p g t", g=8)
        yT = ca.take(2048, F32, "p (g t) -> p g t", g=4)
        ysq = ca.take(2048, F32, "p (g t) -> p g t", g=4)
        m1_off = ca.off
        m1 = ca.take(2048, BF16, "p (g t) -> p g t", g=8)
        pp0 = ca.take(544, F32)
        pp1 = ca.take(544, F32)
        lnm = ca.take(512, F32)
        lnv = ca.take(512, F32)
        lnr = ca.take(512, F32)
        lnt = ca.take(512, F32)
        tmp512 = ca.take(1024, F32)
        junk = ca.take(1024, BF16)
        a1_end = ca.off
        ch = Carver()
        ch.off = m1_off
        xnh = ch.take(1024, BF16)
        hTh = ch.take(256, BF16, "p (k t) -> p k t", k=8)
        sigAh = ch.take(256, F32, "p (g t) -> p g t", g=4)
        gluh_t = ch.take(64, F32)
        assert ch.off <= m1_off + 2048
        cact_rep = RA[:, 0:1024].rearrange("p (k m) -> p k m", k=8)
        cb = Carver()
        qT = cb.take(2048, F32, "p (r q t) -> p r q t", r=2, q=2)
        S_sb = cb.take(4096, F32, "p (g n) -> p g n", g=16)
        work = cb.take(4096, F32, "p (g n) -> p g n", g=16)
        v16 = cb.take(512, F32, "p (g k) -> p g k", g=16)
        idxu = cb.take(512, U32, "p (g k) -> p g k", g=16)
        idxf = cb.take(512, F32, "p (g k) -> p g k", g=16)
        tops = cb.take(256, F32, "p (h k) -> p h k", h=8)
        posu = cb.take(256, U32, "p (h k) -> p h k", h=8)
        apu = cb.take(256, U32, "p (h k) -> p h k", h=8)
        bpu = cb.take(256, U32, "p (h k) -> p h k", h=8)
        apf = cb.take(256, F32, "p (h k) -> p h k", h=8)
        bpf = cb.take(256, F32, "p (h k) -> p h k", h=8)
        areal = cb.take(256, F32, "p (h k) -> p h k", h=8)
        breal = cb.take(256, F32, "p (h k) -> p h k", h=8)
        dd = cb.take(256, F32, "p (h k) -> p h k", h=8)
        ee = cb.take(256, F32, "p (h k) -> p h k", h=8)
        gate = cb.take(256, F32, "p (h k) -> p h k", h=8)
        zz = cb.take(32, F32)
        assert cb.off <= a1_end - 2048, (cb.off, a1_end)
        S_flat = S_sb.rearrange("p g n -> p (g n)")
        W_flat = work.rearrange("p g n -> p (g n)")
        cand = S_flat.rearrange("p (h a b) -> p h a b", h=8, a=16)
        work2 = W_flat.rearrange("p (h n) -> p h n", h=8)
        eq4 = S_flat.rearrange("p (h k a) -> p h k a", h=8, k=16)
        prod4 = W_flat.rearrange("p (h k a) -> p h k a", h=8, k=16)
        xh_sb = xbuf[:, 1, 0, :]
        GT = R[:, :].rearrange("p (t c) -> p t c", c=128)

        bankA = [ps_t("bankA%d" % i, [128, 512], F32) for i in range(4)]
        accB = [ps_t("accB%d" % i, [128, 512], F32) for i in range(4)]
        slotsE = [0, 1]
        slotsA = [2, 3]
        rot = {"E": 0, "A": 0}

        def ps_alloc(kind):
            lst = slotsE if kind == "E" else slotsA
            i = lst[rot[kind] % len(lst)]
            rot[kind] += 1
            return bankA[i]

        for i in range(NSLOT):
            S.new_sem("ld%d" % i)
            S.new_sem("lds%d" % i)
            S.new_sem("wb%d" % i)
        for n in ["setup", "setup2", "xld0", "xld1", "xst", "xh"]:
            S.new_sem(n)

        stream = [] if RECORD else list(stream_in)
        st_state = {"issued": 0, "cur": 0}
        free_slots = list(range(NSLOT))
        slot_of = {}
        load_ev = {}
        wb_ev = {}

        def issue_one(si):
            item = stream[si]
            slot = free_slots.pop(0)
            slot_of[si] = slot
            dst = ring[:, slot, :]
            if item[0] == 'ada':
                ev = S.dma('pool', dst, adaw_d[item[1]], "ld%d" % slot)
            elif item[1] == 0:
                ev = S.dma('pool', dst, wsrc_d[item[2]], "ld%d" % slot)
                wb_ev[item[2]] = S.dma('sp', wscr_d[item[2]], dst, "wb%d" % slot, deps=[ev])
            else:
                ev = S.dma('sp', dst, wscr_d[item[2]], "lds%d" % slot, deps=[wb_ev[item[2]]] if item[1] == 1 else [])
            load_ev[si] = ev

        def try_issue():
            while st_state["issued"] < len(stream) and free_slots:
                issue_one(st_state["issued"])
                st_state["issued"] += 1

        def acquire(item):
            si = st_state["cur"]
            st_state["cur"] += 1
            if RECORD:
                stream.append(item)
                if not free_slots:
                    raise AssertionError("ring exhausted in record mode")
                issue_one(si)
                st_state["issued"] = si + 1
            else:
                assert stream[si] == item, (si, stream[si], item)
                if si not in load_ev:
                    try_issue()
                assert si in load_ev, ("ring slot not available", si, item)
            return ring[:, slot_of[si], :], load_ev[si], si

        def release(si):
            free_slots.append(slot_of[si])
            if not RECORD:
                try_issue()

        def mm(out, lhsT, rhs, start, stop, deps=(), signal=False):
            return S.op('pe', lambda e: e.matmul(out=out, lhsT=lhsT, rhs=rhs, start=start, stop=stop), deps, signal)

        A = lambda **kw: (lambda e: e.activation(**kw))

        S.dma('act', smallp[:], sp_d, "setup2")
        e_setup = S.dma('act', rows_bc[:], rows_d.partition_broadcast(128), "setup")
        if not RECORD:
            try_issue()
        xld_ev = {0: S.dma('act', xbuf[:, 0, :, :], x_d[0:TB, :].rearrange("(t p) d -> p t d", p=128), "xld0")}
        e_xh = S.dma('act', xh_sb[0:32, :], xh_d, "xh")
        xst_ev = {}

        S.op('dve', lambda e: e.tensor_copy(out=identb[:], in_=ident_f), deps=[e_setup])
        S.op('dve', lambda e: e.tensor_copy(out=iota_b[:], in_=iota_f), deps=[e_setup])
        S.op('dve', lambda e: e.memset(ap=ones512[:], constant=1.0 / 512))
        S.op('dve', lambda e: e.memset(ap=epst[:], constant=EPS))
        S.op('act', A(out=cact_f[:], in_=col("c", 0, 8), func=AF.Silu), deps=[e_setup])
        S.op('dve', lambda e: e.tensor_copy(out=cact_bf[:], in_=cact_f[:].unsqueeze(2).to_broadcast([128, 8, 2])))
        S.op('dve', lambda e: e.tensor_copy(out=cact_rep, in_=cact_f[:].unsqueeze(2).to_broadcast([128, 8, 128])))

        modps = ps_alloc("A")
        for j in range(12):
            slot, lev, si = acquire(('ada', j))
            sl = slot.rearrange("p (k n) -> p k n", k=8)
            last = None
            for dt_ in range(4):
                for kt in range(8):
                    last = mm(modps[:, 2 * (j * 4 + dt_):2 * (j * 4 + dt_) + 2], sl[:, kt, dt_ * 128:(dt_ + 1) * 128],
                              cact_bf[:, kt, :], kt == 0, kt == 7, deps=[lev], signal=(kt == 7 and dt_ == 3))
            if j in (4, 5, 10, 11):
                half = j % 2
                which = 1 if j < 8 else 2
                bank = accB[(0 if j < 8 else 2) + half]
                for kt in range(8):
                    last = mm(bank[:, :], cact_rep[:, kt, :], sl[:, kt, :], kt == 0, kt == 7, signal=(kt == 7))
                dst = rows_bc[:, which * 1024 + half * 512: which * 1024 + (half + 1) * 512]
                S.op('dve', lambda e, dst=dst, bank=bank: e.tensor_tensor(out=dst, in0=dst, in1=bank[:, :], op=ALU.add))
            release(si)
        S.op('dve', lambda e: e.tensor_tensor(out=modT[:], in0=modps[:, 0:96].rearrange("p (j two) -> p j two", two=2)[:, :, 0],
                                              in1=col("adab", 0, 48), op=ALU.add))
        S.op('dve', lambda e: e.tensor_single_scalar(out=gtmp, in_=modT[:, 8:16], scalar=1.0, op=ALU.add))
        S.op('dve', lambda e: e.tensor_tensor(out=g1s, in0=gtmp, in1=col("n1g", 0, 8), op=ALU.mult))
        S.op('dve', lambda e: e.tensor_single_scalar(out=gtmp, in_=modT[:, 32:40], scalar=1.0, op=ALU.add))
        S.op('dve', lambda e: e.tensor_tensor(out=g2s, in0=gtmp, in1=col("n2g", 0, 8), op=ALU.mult))

        def rms_and_transpose(xt_aps, npart, dst_hT_fn, gsc, shf, xn_aps, scol=0):
            for ti, xt in enumerate(xt_aps):
                ssc = stat[0:npart, scol + ti:scol + ti + 1]
                sdc = stat[0:npart, 4 + scol + ti:5 + scol + ti]
                rsc = stat[0:npart, 8 + scol + ti:9 + scol + ti]
                S.op('act', A(out=junk[0:npart, :], in_=xt, func=AF.Square, accum_out=ssc))
                S.op('act', A(out=sdc, in_=ssc, func=AF.Sqrt, scale=1.0 / 1024, bias=epst[0:npart, :]))
                yield
                S.op('dve', lambda e: e.reciprocal(out=rsc, in_=sdc))
                xna = xn_aps[ti]
                S.op('dve', lambda e: e.tensor_scalar(out=xna, in0=xt, scalar1=rsc, scalar2=None, op0=ALU.mult))
                yield
                for hf in range(2):
                    pst = ps_alloc("A")[:, 0:256].bitcast(BF16)
                    for q in range(4):
                        kt = hf * 4 + q
                        S.op('pe', lambda e, kt=kt, q=q: e.transpose(
                            out=pst[:, q * 128:q * 128 + npart], in_=xna[:, kt * 128:(kt + 1) * 128],
                            identity=identb[0:npart, 0:npart]), signal=(q == 3))
                    yield
                    for q in range(4):
                        kt = hf * 4 + q
                        S.op('dve', lambda e, kt=kt, q=q: e.tensor_scalar(
                            out=dst_hT_fn(kt, ti), in0=pst[:, q * 128:q * 128 + npart], scalar1=gsc[:, kt:kt + 1],
                            scalar2=shf[:, kt:kt + 1], op0=ALU.mult, op1=ALU.add))
                    yield

        dr = {"i": 0}

        def stageA(b):
            par = b % 2
            xt = [xbuf[:, par, 0, :], xbuf[:, par, 1, :]]
            hT = hTb[:, par, :, :]
            if b == 0:
                yield from rms_and_transpose([xh_sb[0:32, :]], 32, lambda kt, ti: hTh[:, kt, :], g1s, sh1, [xnh[0:32, :]], scol=2)
            yield from rms_and_transpose(xt, 128, lambda kt, ti: hT[:, kt, ti * 128:(ti + 1) * 128], g1s, sh1,
                                         [xn[:, 0, :], xn[:, 1, :]])
            if b > 0:
                S.op('dve', lambda e: e.tensor_copy(out=pT[:, :, 0:16], in_=pmarg[:]))
                S.op('dve', lambda e: e.tensor_copy(out=gluT[:, :, 0:32], in_=gmarg[:]))
            for piece in range(7):
                slot, lev, si = acquire(('w', b, piece))
                sl = slot.rearrange("p (k n) -> p k n", k=8)
                for nl in range(4):
                    if piece == 0:
                        tn = nl
                    elif piece == 1:
                        tn = 8 + nl
                    elif piece == 2:
                        tn = 4 + nl
                    else:
                        tn = 12 + (piece - 3) * 4 + nl
                    bcol = col("bin", tn)
                    if b == 0 and piece < 3:
                        psh = ps_alloc("A")
                        for kt in range(8):
                            mm(psh[:, 0:32], sl[:, kt, nl * 128:(nl + 1) * 128], hTh[:, kt, :], kt == 0, kt == 7,
                               deps=[lev], signal=(kt == 7))
                        if piece == 0:
                            S.op('dve', lambda e: e.tensor_scalar(out=pT[:, nl, 0:16], in0=psh[:, 16:32], scalar1=bcol,
                                                                  scalar2=flag, op0=ALU.add, op1=ALU.mult))
                        elif piece == 1:
                            S.op('act', A(out=sigAh[:, nl, :], in_=psh[:, 0:32], func=AF.Sigmoid, bias=bcol))
                        else:
                            S.op('dve', lambda e: e.scalar_tensor_tensor(out=gluh_t, in0=psh[:, 0:32], scalar=bcol,
                                                                          in1=sigAh[:, nl, :], op0=ALU.add, op1=ALU.mult))
                            S.op('dve', lambda e: e.tensor_scalar(out=gluT[:, nl, 0:32], in0=gluh_t, scalar1=flag,
                                                                  scalar2=None, op0=ALU.mult))
                    psm = ps_alloc("A")[:, 0:256]
                    for kt in range(8):
                        mm(psm, sl[:, kt, nl * 128:(nl + 1) * 128], hT[:, kt, :], kt == 0, kt == 7, deps=[lev], signal=(kt == 7))
                    if piece == 0:
                        S.op('act', A(out=pT[:, nl, 16:272], in_=psm, func=AF.Identity, bias=bcol))
                    elif piece == 1:
                        S.op('act', A(out=sigA[:, nl, :], in_=psm, func=AF.Sigmoid, bias=bcol))
                    elif piece == 2:
                        S.op('dve', lambda e: e.scalar_tensor_tensor(out=gluT[:, nl, 32:288], in0=psm, scalar=bcol,
                                                                      in1=sigA[:, nl, :], op0=ALU.add, op1=ALU.mult))
                    else:
                        n = (piece - 3) * 4 + nl
                        S.op('act', A(out=sgT[:, n, :], in_=psm, func=AF.Sigmoid, bias=bcol))
                    yield
                release(si)
            if b + 1 < NB:
                S.op('dve', lambda e: e.tensor_copy(out=pmarg[:], in_=pT[:, :, 256:272]))
                S.op('dve', lambda e: e.tensor_copy(out=gmarg[:], in_=gluT[:, :, 256:288]))
            for g in range(4):
                src = pT[:, g, :]
                lo = 0
                bufs = [pp0, pp1]
                bi = 0
                for stp in [1, 2, 4, 8][:g + 1]:
                    nlo = lo + stp
                    dstb = bufs[bi]
                    S.op('dve', lambda e, src=src, dstb=dstb, nlo=nlo, stp=stp: e.tensor_tensor(
                        out=dstb[:, nlo:272], in0=src[:, nlo:272], in1=src[:, nlo - stp:272 - stp], op=ALU.add))
                    src = dstb
                    lo = nlo
                    bi ^= 1
                    if stp >= 2:
                        yield
                w = 2 ** (g + 1)
                S.op('dve', lambda e, src=src, g=g, w=w: e.scalar_tensor_tensor(
                    out=mixedT[:, g, :], in0=src[:, 16:272], scalar=1.0 / w, in1=pT[:, g, 16:272], op0=ALU.mult, op1=ALU.subtract))
                if b == 0:
                    S.op('dve', lambda e, src=src, g=g: e.tensor_tensor(out=lnt[:, 0:16], in0=src[:, 16:32], in1=invc[:, g, :], op=ALU.mult))
                    S.op('dve', lambda e, g=g: e.tensor_tensor(out=mixedT[:, g, 0:16], in0=lnt[:, 0:16], in1=pT[:, g, 16:32], op=ALU.subtract))
                yield
            slot, lev, si = acquire(('w', b, 7))
            for g in range(4):
                psm = ps_alloc("A")[:, 0:256]
                mm(psm, slot[:, g * 128:(g + 1) * 128], mixedT[:, g, :], True, True, deps=[lev], signal=True)
                S.op('act', A(out=mix2T[:, g, :], in_=psm, func=AF.Identity, scale=col("psc", g)))
                yield
            release(si)
            taps = [(c, k) for c in range(4) for k in range(31)]
            groups = [taps[i:i + 4] for i in range(0, 124, 4)]
            tile_of = {}

            def build_diag(g):
                for (c, k) in groups[g]:
                    di = dr["i"] % 8
                    dr["i"] += 1
                    tile_of[(c, k)] = di
                    S.op('dve', lambda e, c=c, k=k, di=di: e.tensor_scalar(
                        out=dring[:, di, :], in0=identb[:], scalar1=col("dw", c * 31 + k), scalar2=None, op0=ALU.mult))

            build_diag(0)
            yield
            cps = {}
            for g in range(len(groups)):
                if g + 1 < len(groups):
                    build_diag(g + 1)
                for (c, k) in groups[g]:
                    if k == 0:
                        cps[c] = ps_alloc("A")[:, 0:256]
                    mm(cps[c], dring[:, tile_of[(c, k)], :], gluT[:, c, 2 + k:2 + k + 256], k == 0, k == 30, signal=True)
                    if k == 30:
                        S.op('act', A(out=yT[:, c, :], in_=cps[c], func=AF.Identity, bias=col("dwb", c)))
                        S.op('act', A(out=ysq[:, c, :], in_=cps[c], func=AF.Square, bias=col("dwb", c)))
                yield
            psmean = ps_alloc("A")[:, 0:256]
            for c in range(4):
                mm(psmean, ones512[:], yT[:, c, :], c == 0, c == 3, signal=(c == 3))
            psq = ps_alloc("A")[:, 0:256]
            for c in range(4):
                mm(psq, ones512[:], ysq[:, c, :], c == 0, c == 3, signal=(c == 3))
            yield
            S.op('act', A(out=lnm, in_=psmean, func=AF.Copy))
            yield
            S.op('dve', lambda e: e.tensor_tensor(out=lnt, in0=lnm, in1=lnm, op=ALU.mult))
            S.op('dve', lambda e: e.tensor_tensor(out=lnv, in0=psq, in1=lnt, op=ALU.subtract))
            yield
            S.op('act', A(out=lnv, in_=lnv, func=AF.Sqrt, bias=epst[:, :]))
            yield
            S.op('dve', lambda e: e.reciprocal(out=lnr, in_=lnv))
            yield
            lnts = [lnt, pp0[:, 0:256]]
            for c in range(4):
                lt_ = lnts[c % 2]
                S.op('dve', lambda e, c=c: e.tensor_tensor(out=lt_, in0=yT[:, c, :], in1=lnm, op=ALU.subtract))
                S.op('dve', lambda e: e.tensor_tensor(out=lt_, in0=lt_, in1=lnr, op=ALU.mult))
                yield
                S.op('act', A(out=swT[:, c, :], in_=lt_, func=AF.Silu, scale=col("lng", c), bias=col("lnb", c)))
                yield
            slot, lev, si = acquire(('w', b, 8))
            sl = slot.rearrange("p (k n) -> p k n", k=4)
            for n in range(8):
                psm = ps_alloc("A")[:, 0:256]
                for kt in range(4):
                    mm(psm, sl[:, kt, n * 128:(n + 1) * 128], mix2T[:, kt, :], kt == 0, kt == 3, deps=[lev], signal=(kt == 3))
                S.op('dve', lambda e, n=n: e.tensor_tensor(out=m1[:, n, :], in0=psm, in1=sgT[:, n, :], op=ALU.mult))
                yield
            release(si)
            slot, lev, si = acquire(('w', b, 9))
            sl = slot.rearrange("p (k n) -> p k n", k=4)
            for n in range(8):
                psm = ps_alloc("A")[:, 0:256]
                for kt in range(4):
                    mm(psm, sl[:, kt, n * 128:(n + 1) * 128], swT[:, kt, :], kt == 0, kt == 3, deps=[lev], signal=(kt == 3))
                S.op('dve', lambda e, n=n: e.tensor_tensor(out=lnt, in0=psm, in1=sgT[:, 8 + n, :], op=ALU.mult))
                S.op('dve', lambda e, n=n: e.tensor_tensor(out=mergedT[:, n, :], in0=lnt, in1=m1[:, n, :], op=ALU.add))
                yield
            release(si)
            for half in range(2):
                slot, lev, si = acquire(('w', b, 10 + half))
                sl = slot.rearrange("p (k n) -> p k n", k=8)
                for tt in range(2):
                    pso = ps_alloc("A")
                    for kt in range(8):
                        mm(pso[:, :], mergedT[:, kt, tt * 128:(tt + 1) * 128], sl[:, kt, :], kt == 0, kt == 7, deps=[lev], signal=(kt == 7))
                    S.op('dve', lambda e, half=half: e.tensor_tensor(
                        out=tmp512, in0=pso[:, :], in1=gate1_bc[:, half * 512:(half + 1) * 512], op=ALU.mult))
                    xs = xt[tt][:, half * 512:(half + 1) * 512]
                    S.op('dve', lambda e, xs=xs: e.tensor_tensor(out=xs, in0=xs, in1=tmp512, op=ALU.add))
                    yield
                release(si)
            yield from rms_and_transpose(xt, 128, lambda kt, ti: hT[:, kt, ti * 128:(ti + 1) * 128], g2s, sh2,
                                         [xn[:, 0, :], xn[:, 1, :]])
            def emit_scores(h, r):
                for tt in range(2):
                    pss = ps_alloc("A")[:, 0:256]
                    for half in range(2):
                        mm(pss[:, half * 128:(half + 1) * 128], qT[:, r, half, tt * 128:(tt + 1) * 128], keysT[:, half, :],
                           True, True, signal=(half == 1))
                    dstS = (S_sb if tt == 0 else work)[:, 2 * h:2 * h + 2, :]
                    S.op('act', A(out=dstS, in_=pss.rearrange("p (g n) -> p g n", g=2), func=AF.Copy))

            prev_q = None
            for piece in range(4):
                slot, lev, si = acquire(('w', b, 12 + piece))
                sl = slot.rearrange("p (k n) -> p k n", k=8)
                for hh in range(2):
                    h = piece * 2 + hh
                    r = h % 2
                    pq = ps_alloc("A")
                    for half in range(2):
                        nl = hh * 2 + half
                        for kt in range(8):
                            mm(pq[:, half * 256:(half + 1) * 256], sl[:, kt, nl * 128:(nl + 1) * 128], hT[:, kt, :],
                               kt == 0, kt == 7, deps=[lev], signal=(kt == 7 and half == 1))
                    S.op('act', A(out=qT[:, r, :, :], in_=pq[:, :].rearrange("p (q t) -> p q t", q=2), func=AF.Copy))
                    if prev_q is not None:
                        yield
                        emit_scores(*prev_q)
                    prev_q = (h, r)
                    yield
                release(si)
            emit_scores(*prev_q)
            yield
            pend_tr = []
            for tt in range(2):
                if tt == 1:
                    for q4 in range(4):
                        S.op('act', A(out=S_flat[:, q4 * 512:(q4 + 1) * 512], in_=W_flat[:, q4 * 512:(q4 + 1) * 512], func=AF.Copy))
                        yield 'D'
                for gi in range(16):
                    S.op('dve', lambda e, gi=gi: e.max(out=v16[:, gi, 0:8], in_=S_sb[:, gi, :]))
                    if gi % 4 == 3:
                        yield 'D'
                for gi in range(16):
                    S.op('dve', lambda e, gi=gi: e.max_index(out=idxu[:, gi, 0:8], in_max=v16[:, gi, 0:8], in_values=S_sb[:, gi, :]))
                    if gi % 4 == 3:
                        yield 'D'
                if tt == 1 and pend_tr:
                    pend_tr.pop(0)()
                    yield 'D'
                for gi in range(16):
                    S.op('dve', lambda e, gi=gi: e.match_replace(out=S_sb[:, gi, :], in_to_replace=v16[:, gi, 0:8],
                                                                 in_values=S_sb[:, gi, :], imm_value=-1e30))
                    if gi % 4 == 3:
                        yield 'D'
                for gi in range(16):
                    S.op('dve', lambda e, gi=gi: e.max(out=v16[:, gi, 8:16], in_=S_sb[:, gi, :]))
                    if gi % 4 == 3:
                        yield 'D'
                for gi in range(16):
                    S.op('dve', lambda e, gi=gi: e.max_index(out=idxu[:, gi, 8:16], in_max=v16[:, gi, 8:16], in_values=S_sb[:, gi, :]))
                    if gi % 4 == 3:
                        yield 'D'
                S.op('dve', lambda e: e.tensor_copy(out=idxf, in_=idxu))
                v4 = v16.rearrange("p (h two) k -> p h two k", two=2)
                i4 = idxf.rearrange("p (h two) k -> p h two k", two=2)
                yield 'D'
                for hp in range(4):
                    hs_ = slice(2 * hp, 2 * hp + 2)
                    S.op('dve', lambda e: e.tensor_tensor(
                        out=cand[:, hs_], in0=v4[:, hs_, 0, :].unsqueeze(3).to_broadcast([128, 2, 16, 16]),
                        in1=v4[:, hs_, 1, :].unsqueeze(2).to_broadcast([128, 2, 16, 16]), op=ALU.add))
                    yield 'D'
                candf = cand.rearrange("p h a b -> p h (a b)")
                for h in range(8):
                    S.op('dve', lambda e, h=h: e.max(out=tops[:, h, 0:8], in_=candf[:, h, :]))
                    if h % 4 == 3:
                        yield 'D'
                for h in range(8):
                    S.op('dve', lambda e, h=h: e.max_index(out=posu[:, h, 0:8], in_max=tops[:, h, 0:8], in_values=candf[:, h, :]))
                    if h % 4 == 3:
                        yield 'D'
                for h in range(8):
                    S.op('dve', lambda e, h=h: e.match_replace(out=candf[:, h, :], in_to_replace=tops[:, h, 0:8],
                                                               in_values=candf[:, h, :], imm_value=-1e30))
                    if h % 4 == 3:
                        yield 'D'
                for h in range(8):
                    S.op('dve', lambda e, h=h: e.max(out=tops[:, h, 8:16], in_=candf[:, h, :]))
                    if h % 4 == 3:
                        yield 'D'
                for h in range(8):
                    S.op('dve', lambda e, h=h: e.max_index(out=posu[:, h, 8:16], in_max=tops[:, h, 8:16], in_values=candf[:, h, :]))
                    if h % 4 == 3:
                        yield 'D'
                S.op('dve', lambda e: e.tensor_single_scalar(out=apu, in_=posu, scalar=4, op=ALU.logical_shift_right))
                S.op('dve', lambda e: e.tensor_single_scalar(out=bpu, in_=posu, scalar=15, op=ALU.bitwise_and))
                S.op('dve', lambda e: e.tensor_copy(out=apf, in_=apu))
                S.op('dve', lambda e: e.tensor_copy(out=bpf, in_=bpu))
                yield 'D'
                io4 = iota16.unsqueeze(1).unsqueeze(1).to_broadcast([128, 8, 16, 16])
                eqv, prv = eq4, eq4
                for (posf, side, dstr) in [(apf, 0, areal), (bpf, 1, breal)]:
                    for hp in range(4):
                        hs_ = slice(2 * hp, 2 * hp + 2)
                        io4h = iota16.unsqueeze(1).unsqueeze(1).to_broadcast([128, 2, 16, 16])
                        S.op('dve', lambda e: e.tensor_tensor(out=eqv[:, hs_], in0=posf[:, hs_].unsqueeze(3).to_broadcast([128, 2, 16, 16]),
                                                              in1=io4h, op=ALU.is_equal))
                        yield 'D'
                        S.op('dve', lambda e: e.tensor_tensor(out=eqv[:, hs_], in0=eqv[:, hs_],
                                                              in1=i4[:, hs_, side, :].unsqueeze(2).to_broadcast([128, 2, 16, 16]), op=ALU.mult))
                        yield 'D'
                        S.op('dve', lambda e: e.tensor_reduce(out=dstr[:, hs_], in_=eqv[:, hs_], axis=AX.X, op=ALU.add))
                        yield 'D'
                S.op('dve', lambda e: e.tensor_tensor(out=dd, in0=tops, in1=tops[:, :, 0:1].to_broadcast([128, 8, 16]), op=ALU.subtract))
                yield 'D'
                S.op('act', A(out=ee, in_=dd, func=AF.Exp))
                yield 'D'
                S.op('dve', lambda e: e.tensor_reduce(out=zz[:, 0:8], in_=ee, axis=AX.X, op=ALU.add))
                S.op('dve', lambda e: e.reciprocal(out=zz[:, 8:16], in_=zz[:, 0:8]))
                S.op('dve', lambda e: e.tensor_tensor(out=gate, in0=ee, in1=zz[:, 8:16].unsqueeze(2).to_broadcast([128, 8, 16]), op=ALU.mult))
                yield 'D'
                def emit_tr(tt=tt):
                    for (srcv, dstv) in [(areal, aT), (breal, bT), (gate, gT)]:
                        pst_ = ps_alloc("A")[:, 0:128]
                        S.op('pe', lambda e, srcv=srcv: e.transpose(out=pst_, in_=srcv.rearrange("p h k -> p (h k)"), identity=ident_f))
                        S.op('act', A(out=dstv[:, tt * 128:(tt + 1) * 128], in_=pst_, func=AF.Copy))
                pend_tr.append(emit_tr)
                yield 'D'
            for f_ in pend_tr:
                f_()
            yield 'D'

        def gbuild(b):
            psm = None
            for t in range(TB):
                ls = t % 4
                S.op('dve', lambda e: e.tensor_scalar(out=Rb[:, ls, :], in0=iota_b[:], scalar1=bT[:, t:t + 1], scalar2=None, op0=ALU.is_equal))
                S.op('dve', lambda e: e.tensor_scalar(out=Lb[:, ls, :], in0=iota_b[:], scalar1=aT[:, t:t + 1], scalar2=gT[:, t:t + 1],
                                                      op0=ALU.is_equal, op1=ALU.mult))
                if t % 4 == 0:
                    psm = bankA[t // 4 % 4]
                mm(psm[:, (t % 4) * 128:(t % 4 + 1) * 128], Rb[:, ls, :], Lb[:, ls, :], True, True, signal=True)
                if t % 4 == 3:
                    t0 = t - 3
                    S.op('act', A(out=GT[:, t0:t0 + 4, :], in_=psm[:, :].rearrange("p (t c) -> p t c", t=4), func=AF.Copy))

        def expert(b, genA):
            hT = hTb[:, b % 2, :, :]
            ev_held = {}

            def emit_U(c):
                gq, ci = c // 4, c % 4
                if ci == 0:
                    ev_held[('U', gq)] = acquire(('w', b, 16 + 2 * gq))
                slot, lev, si = ev_held[('U', gq)]
                su = slot.rearrange("p (c k e) -> p c k e", c=4, k=8)
                psm = ps_alloc("E")[:, 0:256]
                for kt in range(8):
                    mm(psm, su[:, ci, kt, :], hT[:, kt, :], kt == 0, kt == 7, deps=[lev], signal=(kt == 7))
                if ci == 3:
                    release(si)
                g_i = c % 2
                S.op('act', A(out=ga[:, g_i, :], in_=psm, func=AF.Gelu_apprx_tanh))
                w_i = c % 3
                S.op('dve', lambda e: e.tensor_tensor(out=Wt[:, w_i, :], in0=ga[:, g_i, :], in1=GT[:, :, c], op=ALU.mult))

            def emit_V(c):
                gq, ci = c // 4, c % 4
                if ci == 0:
                    ev_held[('V', gq)] = acquire(('w', b, 17 + 2 * gq))
                slot, lev, si = ev_held[('V', gq)]
                sv = slot.rearrange("p (c d) -> p c d", c=4)
                w_i = c % 3
                for tt in range(2):
                    for half in range(2):
                        mm(accB[tt * 2 + half][:, :], Wt[:, w_i, tt * 128:(tt + 1) * 128], sv[:, ci, half * 512:(half + 1) * 512],
                           c == 0, c == 127, deps=[lev], signal=(tt == 1 and half == 1))
                if ci == 3:
                    release(si)

            SKEW = 1
            alive = genA is not None
            for c in range(128 + SKEW):
                if c < 128:
                    emit_U(c)
                if c - SKEW >= 0:
                    emit_V(c - SKEW)
                if alive and c >= 2:
                    pp_, pd_ = 0, 0
                    dcap = 2 if (c % 2 == 0) else 1
                    while pp_ < PCAP and pd_ < dcap:
                        try:
                            tag = next(genA)
                        except StopIteration:
                            alive = False
                            break
                        if tag == 'D':
                            pd_ += 1
                            STEP_COUNT[1] += 1
                        else:
                            pp_ += 1
                            STEP_COUNT[0] += 1
            if alive:
                for _ in genA:
                    pass

        def final(b):
            par = b % 2
            xt = [xbuf[:, par, 0, :], xbuf[:, par, 1, :]]
            for tt in range(2):
                for half in range(2):
                    S.op('dve', lambda e: e.tensor_tensor(out=tmp512, in0=accB[tt * 2 + half][:, :],
                                                          in1=gate2_bc[:, half * 512:(half + 1) * 512], op=ALU.mult))
                    xs = xt[tt][:, half * 512:(half + 1) * 512]
                    S.op('dve', lambda e: e.tensor_tensor(out=xs, in0=xs, in1=tmp512, op=ALU.add))
            for tt in range(2):
                ssc = stat[:, tt:tt + 1]
                sdc = stat[:, 4 + tt:5 + tt]
                rsc = stat[:, 8 + tt:9 + tt]
                S.op('act', A(out=junk[:, :], in_=xt[tt], func=AF.Square, accum_out=ssc))
                S.op('act', A(out=sdc, in_=ssc, func=AF.Sqrt, scale=1.0 / 1024, bias=epst[:, :]))
                S.op('dve', lambda e: e.reciprocal(out=rsc, in_=sdc))
                S.op('dve', lambda e: e.scalar_tensor_tensor(out=xt[tt], in0=xt[tt], scalar=rsc, in1=fg_bc, op0=ALU.mult, op1=ALU.mult))
            xst_ev[b] = S.dma('act', out_d[b * TB:(b + 1) * TB, :].rearrange("(t p) d -> p t d", p=128), xbuf[:, par, :, :], "xst")

        gen0 = stageA(0)
        for _ in gen0:
            pass
        if NB > 1:
            S.dma('act', xbuf[:, 1, :, :], x_d[TB:2 * TB, :].rearrange("(t p) d -> p t d", p=128), "xld1")
        for b in range(NB):
            gbuild(b)
            expert(b, stageA(b + 1) if b + 1 < NB else None)
            final(b)
            if b + 2 < NB:
                S.dma('act', xbuf[:, b % 2, :, :], x_d[(b + 2) * TB:(b + 3) * TB, :].rearrange("(t p) d -> p t d", p=128),
                      "xld%d" % (b % 2))
        S.wait('act', xst_ev[NB - 1])
        for g in sorted(wb_ev)[-NSLOT:]:
            S.wait('sp', wb_ev[g])
    if rec_out is not None:
        rec_out.extend(stream)
    return nc


def _prep_shared(inp):
    f = np.float32
    w_in = np.asarray(inp["w_in"], f)[0]
    groups = np.zeros((NG, 128, 4096), f)

    def kt_layout(w, ktn):
        n = w.shape[1]
        return np.ascontiguousarray(w.reshape(ktn, 128, n).transpose(1, 0, 2)).reshape(128, ktn * n)

    pieces = [w_in[:, 0:512], w_in[:, 1024:1536], w_in[:, 512:1024]] + [w_in[:, 1536 + i * 512:1536 + (i + 1) * 512] for i in range(4)]
    for i, p in enumerate(pieces):
        groups[i] = kt_layout(p, 8)
    pw = np.asarray(inp["pool_w"], f)[0]
    groups[7, :, 0:512] = pw.transpose(1, 0, 2).reshape(128, 512)
    groups[8] = kt_layout(np.asarray(inp["pool_up"], f)[0], 4)
    groups[9] = kt_layout(np.asarray(inp["conv_out"], f)[0], 4)
    wo = np.asarray(inp["w_out"], f)[0]
    groups[10] = kt_layout(wo[:, 0:512], 8)
    groups[11] = kt_layout(wo[:, 512:1024], 8)
    wq = np.asarray(inp["w_query"], f)[0]
    for i in range(4):
        groups[12 + i] = kt_layout(wq[:, i * 512:(i + 1) * 512], 8)
    U = np.asarray(inp["expert_u"], f)[0]
    V = np.asarray(inp["expert_v"], f)[0]
    U5 = U.reshape(32, 4, 128, 8, 128)
    groups[16::2] = U5.transpose(0, 4, 1, 3, 2).reshape(32, 128, 4096)
    V4 = V.reshape(32, 4, 128, 1024)
    groups[17::2] = V4.transpose(0, 2, 1, 3).reshape(32, 128, 4096)
    ada_w = np.asarray(inp["ada_w"], f)[0]
    adaw = np.zeros((12, 128, 4096), f)
    for j in range(12):
        adaw[j] = kt_layout(ada_w[:, j * 512:(j + 1) * 512], 8)
    ada_b = np.asarray(inp["ada_b"], f)[0]
    rows = np.concatenate([np.asarray(inp["final_g"], f), ada_b[2048:3072], ada_b[5120:6144]])[None, :]

    sp = np.zeros((128, NCOL), f)

    def put(name, arr):
        o, w = _cols[name]
        sp[:, o:o + w] = arr.reshape(128, w)

    tcol = lambda vec, n: np.asarray(vec, f).reshape(n, 128).T
    put("n1g", tcol(inp["norm1_g"][0], 8))
    put("n2g", tcol(inp["norm2_g"][0], 8))
    put("bin", tcol(inp["b_in"][0], 28))
    put("psc", tcol(inp["pool_scale"][0], 4))
    dw = np.asarray(inp["dw_kernel"], f)[0]
    put("dw", dw.reshape(31, 4, 128).transpose(2, 1, 0).reshape(128, 124))
    put("dwb", tcol(inp["dw_bias"][0], 4))
    put("lng", tcol(inp["conv_ln_g"][0], 4))
    put("lnb", tcol(inp["conv_ln_b"][0], 4))
    put("adab", tcol(ada_b, 48))
    k1 = np.asarray(inp["sub_keys_1"], f)[0]
    k2 = np.asarray(inp["sub_keys_2"], f)[0]
    put("keys", np.concatenate([k1.T, k2.T], axis=1))
    put("ident", np.eye(128, dtype=f))
    put("iota", np.tile(np.arange(128, dtype=f), (128, 1)))
    put("iota16", np.tile(np.arange(16, dtype=f), (128, 1)))
    return groups, adaw, rows, sp


def _core_inputs(inp, shared, batch, start, ntok):
    f = np.float32
    groups, adaw, rows, sp0 = shared
    x = np.asarray(inp["x"], f)
    sp = sp0.copy()
    o, w = _cols["c"]
    sp[:, o:o + w] = np.asarray(inp["c"], f)[batch].reshape(8, 128).T
    o, _ = _cols["flag"]
    first = (start == 0)
    sp[:, o] = 0.0 if first else 1.0
    o, w = _cols["invc"]
    iv = np.zeros((4, 16), f)
    for g in range(4):
        wd = 2 ** (g + 1)
        for t in range(16):
            iv[g, t] = 1.0 / (min(t + 1, wd) if first else wd)
    sp[:, o:o + w] = np.tile(iv.reshape(1, 64), (128, 1))
    if first:
        xh = np.zeros((32, 1024), f)
    else:
        xh = np.ascontiguousarray(x[batch, start - 32:start])
    return {"x": np.ascontiguousarray(x[batch, start:start + ntok]), "xh": xh, "smallp": sp, "rows": rows,
            "adaw": adaw, "wsrc": groups}


_NC_CACHE = {}


def run(inp, ntok, cores):
    if ntok not in _NC_CACHE:
        rec = []
        build_nc(ntok, None, rec)
        _NC_CACHE[ntok] = build_nc(ntok, rec)
    nc = _NC_CACHE[ntok]
    shared = _prep_shared(inp)
    in_maps = [_core_inputs(inp, shared, bt, stt, ntok) for (bt, stt) in cores]
    res = run_bass_kernel_spmd(nc, in_maps, core_ids=list(range(len(cores))))
    return [r["out"] for r in res.results]


def kernel(**inputs):
    x = np.asarray(inputs["x"])
    B, SEQ, D = x.shape
    ntok = SEQ // 2
    cores = [(bt, h * ntok) for bt in range(B) for h in range(2)]
    outs = run(inputs, ntok, cores)
    out = np.zeros((B, SEQ, D), np.float32)
    for (bt, stt), o in zip(cores, outs):
        out[bt, stt:stt + ntok] = o
    return out
```
